# Optimizing a Trainium2 kernel written in Bass

```python
import math
import jax
import jax.numpy as jnp
from jax import lax
import numpy as np

D_MODEL = 2048
BATCH = 16
SEQ = 2048
DEPTH = 4

N_MIXERS = 4
MIX_WIDTH = D_MODEL
RMS_EPS = 1e-6
NEG_INF = -1e30

S5_GROUP = 16
S5_GROUPS = MIX_WIDTH // S5_GROUP
S5_STATE = 64
S5_DT_MIN = 1e-3
S5_DT_MAX = 1e-1

DIFF_HEADS = 16
DIFF_HEAD_DIM = MIX_WIDTH // (2 * DIFF_HEADS)
Q_BLOCK = 128

MOBA_HEADS = 16
MOBA_HEAD_DIM = MIX_WIDTH // MOBA_HEADS
MOBA_BLOCK = 256
MOBA_TOPK = 3
MOBA_Q_CHUNK = 16

SWA_Q_HEADS = 32
SWA_KV_HEADS = 4
SWA_HEAD_DIM = MIX_WIDTH // SWA_Q_HEADS
SWA_WINDOW = 128

kernel_name = 'hybrid_s5_diff_moba_swa_trunk'


def _n_layers_of(kind):
    return len(range(kind, DEPTH, N_MIXERS))


def _rmsnorm(x, gain):
    x32 = x.astype(jnp.float32)
    y = x32 * lax.rsqrt(jnp.mean(x32 * x32, axis=-1, keepdims=True) + RMS_EPS)
    return (y * gain.astype(jnp.float32)).astype(x.dtype)


def _alibi_slopes(n_heads):
    return 2.0 ** (-8.0 * jnp.arange(1, n_heads + 1, dtype=jnp.float32) / n_heads)


def _cdiag_combine(left, right):
    ar1, ai1, br1, bi1 = left
    ar2, ai2, br2, bi2 = right
    return (ar2 * ar1 - ai2 * ai1,
            ar2 * ai1 + ai2 * ar1,
            ar2 * br1 - ai2 * bi1 + br2,
            ar2 * bi1 + ai2 * br1 + bi2)


def _s5_branch(h, w_in, a_re, a_im, log_dt, b_re, b_im, c_re, c_im, d_skip, w_glu, b_glu, w_out):
    f32 = jnp.float32
    bsz, seq, _ = h.shape
    proj = h @ w_in
    u, z = proj[..., :MIX_WIDTH], proj[..., MIX_WIDTH:]
    u32 = u.astype(f32)
    ug = u32.reshape(bsz, seq, S5_GROUPS, S5_GROUP)
    lr, li = a_re.astype(f32), a_im.astype(f32)
    dt = jnp.exp(log_dt.astype(f32))[:, None]
    mag = jnp.exp(lr * dt)
    ab_re, ab_im = mag * jnp.cos(li * dt), mag * jnp.sin(li * dt)
    den = lr * lr + li * li
    f_re = ((ab_re - 1.0) * lr + ab_im * li) / den
    f_im = (ab_im * lr - (ab_re - 1.0) * li) / den
    br, bi = b_re.astype(f32), b_im.astype(f32)
    bb_re = f_re[..., None] * br - f_im[..., None] * bi
    bb_im = f_re[..., None] * bi + f_im[..., None] * br
    bu_re = jnp.einsum('bsgc,gpc->bsgp', ug, bb_re)
    bu_im = jnp.einsum('bsgc,gpc->bsgp', ug, bb_im)
    shape_a = (1, seq, S5_GROUPS, S5_STATE)
    elems = (jnp.broadcast_to(ab_re, shape_a), jnp.broadcast_to(ab_im, shape_a), bu_re, bu_im)
    _, _, st_re, st_im = lax.associative_scan(_cdiag_combine, elems, axis=1)
    y = (jnp.einsum('bsgp,gcp->bsgc', st_re, c_re.astype(f32))
         - jnp.einsum('bsgp,gcp->bsgc', st_im, c_im.astype(f32)))
    y = y.reshape(bsz, seq, MIX_WIDTH) + d_skip.astype(f32) * u32
    g = jax.nn.gelu(y)
    y = g * jax.nn.sigmoid(g @ w_glu.astype(f32) + b_glu.astype(f32))
    y = y.astype(h.dtype) * jax.nn.silu(z)
    return y @ w_out


def _diff_branch(h, layer_idx, w_in, lq1, lk1, lq2, lk2, subln, w_out):
    f32 = jnp.float32
    bsz, seq, _ = h.shape
    nh, d = DIFF_HEADS, DIFF_HEAD_DIM
    q, k, v, z = jnp.split(h @ w_in, 4, axis=-1)
    q = q.reshape(bsz, seq, nh, 2, d).transpose(3, 0, 2, 1, 4).astype(f32)
    k = k.reshape(bsz, seq, nh, 2, d).transpose(3, 0, 2, 1, 4).astype(f32)
    v = v.reshape(bsz, seq, nh, 2 * d).transpose(0, 2, 1, 3).astype(f32)
    lam_init = 0.8 - 0.6 * math.exp(-0.3 * layer_idx)
    lam = (jnp.exp(jnp.sum(lq1.astype(f32) * lk1.astype(f32)))
           - jnp.exp(jnp.sum(lq2.astype(f32) * lk2.astype(f32))) + lam_init)
    slopes = _alibi_slopes(nh)[:, None, None]
    scale = d ** -0.5
    outs = []
    for start in range(0, seq, Q_BLOCK):
        end = start + Q_BLOCK
        dist = (jnp.arange(start, end)[:, None] - jnp.arange(end)[None, :]).astype(f32)
        bias = jnp.where(dist >= 0, -slopes * dist, NEG_INF)
        s = jnp.einsum('ibhqd,ibhkd->ibhqk', q[:, :, :, start:end], k[:, :, :, :end]) * scale + bias
        a = jax.nn.softmax(s, axis=-1)
        attn = a[0] - lam * a[1]
        outs.append(jnp.einsum('bhqk,bhkd->bhqd', attn, v[:, :, :end]))
    o = jnp.concatenate(outs, axis=2)
    o = _rmsnorm(o, subln) * (1.0 - lam_init)
    o = o.transpose(0, 2, 1, 3).reshape(bsz, seq, MIX_WIDTH)
    y = o.astype(h.dtype) * jax.nn.silu(z)
    return y @ w_out


def _moba_attention(q, k, v):
    f32 = jnp.float32
    bsz, seq, nh, dh = q.shape
    blk = MOBA_BLOCK
    nb = -(-seq // blk)
    sp = nb * blk
    pad = ((0, 0), (0, sp - seq), (0, 0), (0, 0))
    qh = jnp.pad(q, pad).astype(f32).transpose(0, 2, 1, 3)
    kh = jnp.pad(k, pad).astype(f32).transpose(0, 2, 1, 3)
    vh = jnp.pad(v, pad).astype(f32).transpose(0, 2, 1, 3)
    kb = kh.reshape(bsz, nh, nb, blk, dh)
    vb = vh.reshape(bsz, nh, nb, blk, dh)
    scale = dh ** -0.5
    slopes = _alibi_slopes(nh)
    loc = jnp.arange(blk)
    dist_own = (loc[:, None] - loc[None, :]).astype(f32)
    s_own = (jnp.einsum('bhnqd,bhnkd->bhnqk', qh.reshape(bsz, nh, nb, blk, dh), kb) * scale
             - slopes[None, :, None, None, None] * dist_own)
    s_own = jnp.where(dist_own >= 0, s_own, NEG_INF)
    m_own = jnp.max(s_own, axis=-1)
    p_own = jnp.exp(s_own - m_own[..., None])
    l_own = jnp.sum(p_own, axis=-1).reshape(bsz, nh, sp)
    o_own = jnp.einsum('bhnqk,bhnkd->bhnqd', p_own, vb).reshape(bsz, nh, sp, dh)
    m_own = m_own.reshape(bsz, nh, sp)
    n_sel = min(MOBA_TOPK, nb - 1)
    if n_sel == 0:
        out = o_own / l_own[..., None]
    else:
        pos = jnp.arange(sp)
        qblk = pos // blk
        gate = jnp.einsum('bhtd,bhnd->bhtn', qh, jnp.mean(kb, axis=3))
        past = jnp.arange(nb)[None, :] < qblk[:, None]
        gate = jnp.where(past, gate, NEG_INF)
        _, idx = lax.top_k(gate, n_sel)
        valid = jnp.arange(n_sel)[None, :] < qblk[:, None]
        n_chunks = sp // MOBA_Q_CHUNK
        bi = jnp.arange(bsz)[:, None, None, None]
        hi = jnp.arange(nh)[None, :, None, None]

        def chunk(args):
            qc, idxc, validc, tc = args
            kg = kb[bi, hi, idxc]
            vg = vb[bi, hi, idxc]
            dist = (tc[None, None, :, None, None] - (idxc[..., None] * blk + loc)).astype(f32)
            s = (jnp.einsum('bhcd,bhcjkd->bhcjk', qc, kg) * scale
                 - slopes[None, :, None, None, None] * dist)
            mask = validc[None, None, :, :, None]
            s = jnp.where(mask, s, NEG_INF)
            m = jnp.max(s, axis=(-2, -1))
            p = jnp.where(mask, jnp.exp(s - m[..., None, None]), 0.0)
            return m, jnp.sum(p, axis=(-2, -1)), jnp.einsum('bhcjk,bhcjkd->bhcd', p, vg)

        def to_chunks(t):
            return jnp.moveaxis(t.reshape(bsz, nh, n_chunks, MOBA_Q_CHUNK, *t.shape[3:]), 2, 0)

        def from_chunks(t):
            t = jnp.moveaxis(t, 0, 2)
            return t.reshape(bsz, nh, sp, *t.shape[4:])

        xs = (to_chunks(qh), to_chunks(idx),
              valid.reshape(n_chunks, MOBA_Q_CHUNK, n_sel), pos.reshape(n_chunks, MOBA_Q_CHUNK))
        m_sel, l_sel, o_sel = lax.map(chunk, xs)
        m_sel, l_sel, o_sel = from_chunks(m_sel), from_chunks(l_sel), from_chunks(o_sel)
        m = jnp.maximum(m_own, m_sel)
        a_own, a_sel = jnp.exp(m_own - m), jnp.exp(m_sel - m)
        out = ((o_own * a_own[..., None] + o_sel * a_sel[..., None])
               / (l_own * a_own + l_sel * a_sel)[..., None])
    return out[:, :, :seq].transpose(0, 2, 1, 3)


def _moba_branch(h, w_in, w_out):
    bsz, seq, _ = h.shape
    q, k, v, z = jnp.split(h @ w_in, 4, axis=-1)
    shp = (bsz, seq, MOBA_HEADS, MOBA_HEAD_DIM)
    o = _moba_attention(q.reshape(shp), k.reshape(shp), v.reshape(shp))
    y = o.reshape(bsz, seq, MIX_WIDTH).astype(h.dtype) * jax.nn.silu(z)
    return y @ w_out


def _swa_branch(h, w_in, sinks, w_out):
    f32 = jnp.float32
    bsz, seq, _ = h.shape
    hq, hk, dh, win = SWA_Q_HEADS, SWA_KV_HEADS, SWA_HEAD_DIM, SWA_WINDOW
    grp = hq // hk
    nqc, nkc = hq * dh, hk * dh
    proj = h @ w_in
    q = proj[..., :nqc]
    k = proj[..., nqc:nqc + nkc]
    v = proj[..., nqc + nkc:nqc + 2 * nkc]
    z = proj[..., nqc + 2 * nkc:]
    nblk = seq // win
    q = q.reshape(bsz, nblk, win, hk, grp, dh).astype(f32)
    k = k.reshape(bsz, seq, hk, dh).astype(f32)
    v = v.reshape(bsz, seq, hk, dh).astype(f32)

    def band(t):
        tp = jnp.pad(t, ((0, 0), (win, 0), (0, 0), (0, 0)))
        prev = tp[:, :seq].reshape(bsz, nblk, win, hk, dh)
        cur = t.reshape(bsz, nblk, win, hk, dh)
        return jnp.concatenate([prev, cur], axis=2)

    kb, vb = band(k), band(v)
    qi = jnp.arange(win)[:, None]
    kj = jnp.arange(2 * win)[None, :]
    dist = qi + win - kj
    kpos = jnp.arange(nblk)[:, None, None] * win + kj[None] - win
    valid = (dist >= 0) & (dist < win) & (kpos >= 0)
    slopes = _alibi_slopes(hq).reshape(hk, grp)
    s = (jnp.einsum('bnqkgd,bnjkd->bnkgqj', q, kb) * dh ** -0.5
         - slopes[:, :, None, None] * dist.astype(f32))
    s = jnp.where(valid[None, :, None, None], s, NEG_INF)
    sink = sinks.astype(f32).reshape(hk, grp)[:, :, None]
    m = jnp.maximum(jnp.max(s, axis=-1), sink)
    p = jnp.exp(s - m[..., None])
    denom = jnp.sum(p, axis=-1) + jnp.exp(sink - m)
    o = jnp.einsum('bnkgqj,bnjkd->bnqkgd', p, vb) / denom.transpose(0, 1, 4, 2, 3)[..., None]
    y = o.reshape(bsz, seq, MIX_WIDTH).astype(h.dtype) * jax.nn.silu(z)
    return y @ w_out


def setup_inputs(seed: int = 0) -> dict:
    key = jax.random.key(seed)
    ks = jax.random.split(key, 32)
    f32 = jnp.float32
    d, e = D_MODEL, MIX_WIDTH
    n_a, n_b, n_c, n_d = (_n_layers_of(0), _n_layers_of(1), _n_layers_of(2), _n_layers_of(3))
    g, p, c = S5_GROUPS, S5_STATE, S5_GROUP

    def nrm(k, shape, scale):
        return jax.random.normal(k, shape, f32) * scale

    swa_cols = SWA_Q_HEADS * SWA_HEAD_DIM + 2 * SWA_KV_HEADS * SWA_HEAD_DIM + e
    n_idx = jnp.arange(p, dtype=f32)
    return {
        'x': nrm(ks[0], (BATCH, SEQ, d), 1.0),
        'pre_norm': 1.0 + nrm(ks[1], (DEPTH, d), 0.02),
        'post_norm': 1.0 + nrm(ks[2], (DEPTH, d), 0.02),
        's5_w_in': nrm(ks[3], (n_a, d, 2 * e), d ** -0.5),
        's5_a_re': -0.5 + nrm(ks[4], (n_a, g, p), 0.01),
        's5_a_im': math.pi * n_idx + nrm(ks[5], (n_a, g, p), 0.01),
        's5_log_dt': jax.random.uniform(ks[6], (n_a, g), f32, math.log(S5_DT_MIN), math.log(S5_DT_MAX)),
        's5_b_re': nrm(ks[7], (n_a, g, p, c), (2 * c) ** -0.5),
        's5_b_im': nrm(ks[8], (n_a, g, p, c), (2 * c) ** -0.5),
        's5_c_re': nrm(ks[9], (n_a, g, c, p), 0.5),
        's5_c_im': nrm(ks[10], (n_a, g, c, p), 0.5),
        's5_d': nrm(ks[11], (n_a, e), 1.0),
        's5_w_glu': nrm(ks[12], (n_a, e, e), e ** -0.5),
        's5_b_glu': nrm(ks[13], (n_a, e), 0.01),
        's5_w_out': nrm(ks[14], (n_a, e, d), e ** -0.5),
        'diff_w_in': nrm(ks[15], (n_b, d, 4 * e), d ** -0.5),
        'diff_lq1': nrm(ks[16], (n_b, DIFF_HEAD_DIM), 0.1),
        'diff_lk1': nrm(ks[17], (n_b, DIFF_HEAD_DIM), 0.1),
        'diff_lq2': nrm(ks[18], (n_b, DIFF_HEAD_DIM), 0.1),
        'diff_lk2': nrm(ks[19], (n_b, DIFF_HEAD_DIM), 0.1),
        'diff_subln': 1.0 + nrm(ks[20], (n_b, 2 * DIFF_HEAD_DIM), 0.02),
        'diff_w_out': nrm(ks[21], (n_b, e, d), e ** -0.5),
        'moba_w_in': nrm(ks[22], (n_c, d, 4 * e), d ** -0.5),
        'moba_w_out': nrm(ks[23], (n_c, e, d), e ** -0.5),
        'swa_w_in': nrm(ks[24], (n_d, d, swa_cols), d ** -0.5),
        'swa_sinks': nrm(ks[25], (n_d, SWA_Q_HEADS), 0.5),
        'swa_w_out': nrm(ks[26], (n_d, e, d), e ** -0.5),
    }


def reference(x, pre_norm, post_norm,
              s5_w_in, s5_a_re, s5_a_im, s5_log_dt, s5_b_re, s5_b_im, s5_c_re, s5_c_im,
              s5_d, s5_w_glu, s5_b_glu, s5_w_out,
              diff_w_in, diff_lq1, diff_lk1, diff_lq2, diff_lk2, diff_subln, diff_w_out,
              moba_w_in, moba_w_out,
              swa_w_in, swa_sinks, swa_w_out):
    for i in range(DEPTH):
        kind, j = i % N_MIXERS, i // N_MIXERS
        h = _rmsnorm(x, pre_norm[i])
        if kind == 0:
            y = _s5_branch(h, s5_w_in[j], s5_a_re[j], s5_a_im[j], s5_log_dt[j], s5_b_re[j],
                           s5_b_im[j], s5_c_re[j], s5_c_im[j], s5_d[j], s5_w_glu[j],
                           s5_b_glu[j], s5_w_out[j])
        elif kind == 1:
            y = _diff_branch(h, i, diff_w_in[j], diff_lq1[j], diff_lk1[j], diff_lq2[j],
                             diff_lk2[j], diff_subln[j], diff_w_out[j])
        elif kind == 2:
            y = _moba_branch(h, moba_w_in[j], moba_w_out[j])
        else:
            y = _swa_branch(h, swa_w_in[j], swa_sinks[j], swa_w_out[j])
        x = x + _rmsnorm(y, post_norm[i])
    return x
```

```python
import math
from contextlib import ExitStack

import numpy as np
import concourse.bass as bass
import concourse.mybir as mybir
from concourse.bass_utils import run_bass_kernel_spmd

F32 = mybir.dt.float32
BF16 = mybir.dt.bfloat16
I32 = mybir.dt.int32
AF = mybir.ActivationFunctionType
ALU = mybir.AluOpType
AX = mybir.AxisListType

D = 2048
S = 2048
NT = 16
NC = 16
EPS = 1e-6
BIG = 60000.0
ENG = ("pe", "act", "dve", "pool", "sp")
DEBUG_STOP = [0, 16, 0]


class Buf:
    __slots__ = ("w", "r", "name")

    def __init__(self, name=""):
        self.w = None
        self.r = {}
        self.name = name


class Prog:
    NDMA = 24

    def __init__(self):
        self.ops = []
        self.cnt = {e: 0 for e in ENG}
        self.waited = {e: {} for e in ENG}
        self.dcnt = [0] * self.NDMA
        self.drr = 0
        self.fence_ev = {}

    def fence(self):
        ev = {e: self.cnt[e] for e in ENG if self.cnt[e]}
        for d in range(self.NDMA):
            if self.dcnt[d]:
                ev[("d", d)] = 16 * self.dcnt[d]
        self.fence_ev = ev

    def _deps(self, eng, reads, writes, extra=()):
        need = {}

        def add(ev):
            if ev is None:
                return
            k, v = ev
            if need.get(k, 0) < v:
                need[k] = v

        for b in reads:
            add(b.w)
        for b in writes:
            add(b.w)
            for k, v in b.r.items():
                add((k, v))
        for ev in extra:
            add(ev)
        for k, v in self.fence_ev.items():
            add((k, v))
        wl = []
        for k, v in need.items():
            if k == "pe" and eng == "pe":
                continue
            if self.waited[eng].get(k, 0) >= v:
                continue
            self.waited[eng][k] = v
            wl.append((k, v))
        return wl

    def op(self, eng, fn, reads=(), writes=()):
        wl = self._deps(eng, reads, writes)
        self.cnt[eng] += 1
        ev = (eng, self.cnt[eng])
        self.ops.append((eng, fn, wl, ev, 1))
        for b in reads:
            if b.r.get(eng, 0) < ev[1]:
                b.r[eng] = ev[1]
        for b in writes:
            b.w = ev
            b.r = {}
        return ev

    def dma(self, eng, fn, reads=(), writes=()):
        d = self.drr
        self.drr = (self.drr + 1) % self.NDMA
        key = ("d", d)
        extra = [(key, 16 * self.dcnt[d])] if self.dcnt[d] else []
        wl = self._deps(eng, reads, writes, extra)
        self.dcnt[d] += 1
        ev = (key, 16 * self.dcnt[d])
        self.ops.append((eng, fn, wl, ev, 16))
        for b in reads:
            b.r[key] = ev[1]
        for b in writes:
            b.w = ev
            b.r = {}
        return ev

    def emit(self, nc, block, sems):
        engmap = {"pe": block.tensor, "act": block.scalar, "dve": block.vector,
                  "pool": block.gpsimd, "sp": block.sync}
        for en in ENG:
            ops = [o for o in self.ops if o[0] == en]
            final = []
            if en == "sp":
                for d in range(self.NDMA):
                    if self.dcnt[d]:
                        final.append((("d", d), 16 * self.dcnt[d]))
                for e2 in ENG:
                    if e2 != "sp" and self.cnt[e2]:
                        final.append((e2, self.cnt[e2]))

            def body(e, ops=ops, final=final):
                for (_, fn, wl, ev, amt) in ops:
                    for k, v in wl:
                        e.wait_ge(sems[k], v)
                    ins = fn(e)
                    ins.then_inc(sems[ev[0]], amt)
                for k, v in final:
                    e.wait_ge(sems[k], v)

            engmap[en](body)


def alibi_slopes(n):
    return [2.0 ** (-8.0 * (h + 1) / n) for h in range(n)]


def build_program(NSEQ, LAYERS):
    nc = bass.Bass("TRN2", target_bir_lowering=False)
    P = Prog()

    def din(name, shape):
        return nc.dram_tensor(name, list(shape), F32, kind="ExternalInput").ap()

    x_in = din("x", [NSEQ, S, D])
    out = nc.dram_tensor("out", [NSEQ, S, D], F32, kind="ExternalOutput").ap()
    pre_gT = din("pre_gT", [4, 128, NC])
    post_g = din("post_norm", [4, D])
    W = {}
    W["s5_w_in"] = din("s5_w_in", [D, 2 * D])
    W["s5_w_glu"] = din("s5_w_glu", [D, D])
    W["s5_w_out"] = din("s5_w_out", [D, D])
    W["diff_w_in"] = din("diff_w_in", [D, 4 * D])
    W["diff_w_out"] = din("diff_w_out", [D, D])
    W["moba_w_in"] = din("moba_w_in", [D, 4 * D])
    W["moba_w_out"] = din("moba_w_out", [D, D])
    W["swa_w_in"] = din("swa_w_in", [D, 4608])
    W["swa_w_out"] = din("swa_w_out", [D, D])
    diff_l = din("diff_l", [4, 64])
    diff_subln = din("diff_subln", [1, 128])
    swa_sinks = din("swa_sinks", [1, 32])
    s5p = {}
    for nm, shp in (("s5_are_X", [128, 16, 64]), ("s5_aim_X", [128, 16, 64]), ("s5_ldt_X", [128, 16]),
                    ("s5_bre_X", [128, 16, 64]), ("s5_bim_X", [128, 16, 64]),
                    ("s5_are_Y", [128, 64]), ("s5_aim_Y", [128, 64]), ("s5_ldt_Y", [128, 64]),
                    ("s5_cre_Y", [128, 64, 16]), ("s5_cim_Y", [128, 64, 16]),
                    ("s5_dT", [128, NC]), ("s5_b_gluT", [128, NC])):
        s5p[nm] = din(nm, shp)

    SZ = nc.dram_tensor("scr_sz", [S, D], BF16).ap()
    YS = nc.dram_tensor("scr_y", [S, D], BF16).ap()
    ZT = nc.dram_tensor("scr_zt", [NC, 128, S], BF16).ap()

    es = ExitStack()

    def sb(name, shape, dt):
        return es.enter_context(nc.sbuf_tensor(name, list(shape), dt))

    def ps(name, shape, dt):
        return es.enter_context(nc.psum_tensor(name, list(shape), dt))

    R1 = sb("R1", [128, NC, 2048], BF16)
    R2 = sb("R2", [128, 16640], F32)
    WS = [sb(f"WS{i}", [128, NC, 256], BF16) for i in range(2)]
    STG = [sb(f"STG{i}", [128, 8, 256], F32) for i in range(2)]
    QT = [sb("QT0", [128, 2048], BF16)] * 2
    KT = [sb("KT0", [128, 2048], BF16)] * 2
    AUXQ = [sb("AUXQ0", [12, 2048], BF16)] * 2
    AUXK = sb("AUXK", [12, 2048], BF16)
    PT = [sb(f"PT{i}", [128, 512], BF16) for i in range(3)]
    IDENT = sb("IDENT", [128, 128], BF16)
    CM = sb("CM", [128, 128], BF16)
    CM2 = sb("CM2", [128, 128], BF16)
    IOD = sb("IOD", [128, 128], F32)
    GT = sb("GT", [128, 4, NC], F32)
    SMALL = sb("SMALL", [128, 256], F32)
    SUBG = sb("SUBG", [128, 128], F32)
    LAMW = sb("LAMW", [128, 4, 64], F32)
    ESINK = sb("ESINK", [128, 32], F32)
    OW = [sb(f"OW{i}", [128, 128], F32) for i in range(2)]
    OW2 = [sb(f"OW2{i}", [128, 128], F32) for i in range(2)]
    YB = [sb(f"YB{i}", [128, 4, 128], BF16) for i in range(2)]
    SZS = [sb(f"SZS{i}", [128, 512], BF16) for i in range(2)]
    GATE = sb("GATE", [128, 16, 8], F32)
    GATE2 = sb("GATE2", [128, 16, 8], F32)
    MASKV = sb("MASKV", [128, 16, 8], BF16)
    TOP8 = sb("TOP8", [128, 16, 8], F32)
    PASTM = sb("PASTM", [128, 16, 8], F32)
    PAST01 = sb("PAST01", [128, 16, 8], F32)
    OWN01 = sb("OWN01", [128, 16, 8], F32)
    KMEAN = sb("KMEAN", [128, 8], F32)
    KMEANB = sb("KMEANB", [128, 8], BF16)
    OWG = [sb(f"OWG{i}", [128, 512], F32) for i in range(2)]
    bOWG = [Buf(), Buf()]
    S5P = sb("S5P", [128, 8, 64], F32)
    S5D = sb("S5D", [128, 2, NC], F32)
    CAR = sb("CAR", [128, 8], F32)
    MSK = sb("MSK", [128, 8], F32)

    psA = [ps(f"psA{i}", [128, 512], F32) for i in range(2)]
    psG = [ps(f"psG{i}", [128, 512], F32) for i in range(1)]
    psO = ps("psO", [128, 4, 512], F32)
    psT = [ps(f"psT{i}", [128, 8, 128], BF16) for i in range(1)]

    bA = [Buf("psA0"), Buf("psA1")]
    bG = [Buf("psG0")]
    bT = [Buf("psT0")]
    bO = [[Buf(f"psO{j}")] * 2 for j in range(4)]
    bOall = Buf("psOall")

    R2b = R2[:, :].bitcast(BF16)
    VP = R2b[:, 0:16 * 16 * 129].rearrange("p (t h d) -> p t h d", t=16, h=16)
    VPs = R2b[:, 0:16 * 4 * 65].rearrange("p (t h d) -> p t h d", t=16, h=4)
    XT = [R2[:, 0:2048], R2[:, 2048:4096]]
    XN = [R2b[:, 8192:10240], R2b[:, 10240:12288]]
    JUNK = R2b[:, 12288:14336]
    PG = R2[:, 7168:9216]
    TT = R2[:, 9216:11264]
    Yt = [R2b[:, 22528:24576], R2b[:, 24576:26624]]
    SZt = [R2b[:, 26624:28672], R2b[:, 28672:30720]]
    YG = R2b[:, 30720:32768]
    YGT = [R2b[:, 32768:33024].rearrange("p (c t) -> p c t", c=2)]
    bXT = [Buf(), Buf()]
    bXN = [Buf(), Buf()]
    bJ = Buf()
    bPG = Buf()
    bTT = Buf()
    bYt = [Buf(), Buf()]
    bSZt = [Buf(), Buf()]
    bYG = Buf()
    bSS = [Buf(), Buf()]
    YGTv = R2b[:, 8192:10240].rearrange("p (c t) -> p c t", c=16)
    bYGT = Buf()

    bR1 = [Buf(f"R1_{c}") for c in range(NC)]
    bWS = [Buf("WS0"), Buf("WS1")]
    bSTG = [Buf("STG0"), Buf("STG1")]
    bQT = [Buf()] * 2
    bKT = [Buf()] * 2
    bAUXQ = [Buf()] * 2
    bPT = [Buf(), Buf(), Buf()]
    bVP = [Buf(f"VP{t}") for t in range(NT)]
    bOW = [Buf(), Buf()]
    bOW2 = [Buf(), Buf()]
    bYB = [Buf(), Buf()]
    bSZS = [Buf(), Buf()]
    bSM = Buf("small")
    bConst = Buf("const")
    bX = [[Buf(f"x{s}_{i}") for i in range(NT)] for s in range(NSEQ)]
    bSZd = [Buf(f"szd{i}") for i in range(NT)]
    bYd = [Buf(f"yd{i}") for i in range(4)]
    bGate = Buf()
    bMaskv = Buf()

    st = {"ws": 0, "pt": 0, "pa": 0, "ev": 0, "stg": 0}

    C_SS, C_RSTD, C_LAM, C_R0, C_R1, C_SS2, C_TMP = 0, 8, 16, 24, 32, 40, 48

    def evac_engine():
        st["ev"] ^= 1
        return "act" if st["ev"] else "dve"

    def copy_op(eng, out_ap, in_ap, reads, writes, scale=None):
        if eng == "act":
            if scale is None:
                P.op("act", lambda e: e.copy(out=out_ap, in_=in_ap), reads, writes)
            else:
                P.op("act", lambda e: e.mul(out=out_ap, in_=in_ap, mul=float(scale)), reads, writes)
        else:
            if scale is None:
                P.op("dve", lambda e: e.tensor_copy(out=out_ap, in_=in_ap), reads, writes)
            else:
                P.op("dve", lambda e: e.tensor_scalar_mul(out=out_ap, in0=in_ap, scalar1=float(scale)),
                     reads, writes)

    def setup():
        P.op("pool", lambda e: e.iota(IOD[:], pattern=[[1, 128]], base=0, channel_multiplier=-1,
                                      allow_small_or_imprecise_dtypes=True), (), (bConst,))
        P.op("dve", lambda e: e.tensor_single_scalar(out=IDENT[:], in_=IOD[:], scalar=0.0, op=ALU.is_equal),
             (bConst,), (bConst,))
        P.op("dve", lambda e: e.tensor_scalar(out=CM[:], in0=IOD[:], scalar1=0.0, scalar2=-BIG,
                                              op0=ALU.is_lt, op1=ALU.mult), (bConst,), (bConst,))
        P.op("dve", lambda e: e.tensor_scalar(out=CM2[:], in0=IOD[:], scalar1=0.0, scalar2=-BIG,
                                              op0=ALU.is_ge, op1=ALU.mult), (bConst,), (bConst,))
        T_a = R2b[0:1, 0:2048]
        T_b = R2b[0:1, 2048:4096]
        T_1 = R2b[0:1, 4096:6144]
        T_m = R2b[0:1, 6144:8192]
        bt = Buf()
        P.op("pool", lambda e: e.iota(T_a, pattern=[[128, 16], [0, 128]], base=0, channel_multiplier=0,
                                      allow_small_or_imprecise_dtypes=True), (), (bt,))
        P.op("pool", lambda e: e.iota(T_b, pattern=[[0, 16], [1, 128]], base=0, channel_multiplier=0,
                                      allow_small_or_imprecise_dtypes=True), (), (bt,))
        P.op("dve", lambda e: e.memset(T_1, 1.0), (), (bt,))
        P.op("dve", lambda e: e.memset(T_m, -1.0), (), (bt,))
        for r, src in enumerate((T_a, T_b, T_1, T_1)):
            P.dma("sp", lambda e, r=r, src=src: e.dma_start(out=AUXQ[0][8 + r:9 + r, :], in_=src),
                  (bt,), (bConst,))
        for r, src in enumerate((T_m, T_m, T_a, T_b)):
            P.dma("sp", lambda e, r=r, src=src: e.dma_start(out=AUXK[8 + r:9 + r, :], in_=src),
                  (bt,), (bConst,))
        BLK = R2[0:8, 4096:6144]
        BLK2 = R2[0:8, 6144:8192]
        P.op("pool", lambda e: e.iota(BLK, pattern=[[1, 2048]], base=0, channel_multiplier=-256,
                                      allow_small_or_imprecise_dtypes=True), (bt,), (bt,))
        P.op("dve", lambda e: e.tensor_scalar(out=BLK2, in0=BLK, scalar1=0.0, scalar2=None, op0=ALU.is_ge),
             (bt,), (bt,))
        P.op("dve", lambda e: e.tensor_scalar(out=BLK, in0=BLK, scalar1=256.0, scalar2=None, op0=ALU.is_lt),
             (bt,), (bt,))
        P.op("dve", lambda e: e.tensor_tensor(out=AUXK[0:8, :], in0=BLK, in1=BLK2, op=ALU.mult),
             (bt,), (bConst,))
        NB = R2[:, 8192:8320].rearrange("p (t n) -> p t n", t=16)
        TB = R2[:, 8320:8448].rearrange("p (t n) -> p t n", t=16)
        P.op("pool", lambda e: e.iota(NB, pattern=[[0, 16], [1, 8]], base=0, channel_multiplier=0,
                                      allow_small_or_imprecise_dtypes=True), (), (bt,))
        P.op("pool", lambda e: e.iota(TB, pattern=[[1, 16], [0, 8]], base=0, channel_multiplier=0,
                                      allow_small_or_imprecise_dtypes=True), (), (bt,))
        D2 = R2[:, 8448:8576].rearrange("p (t n) -> p t n", t=16)
        P.op("dve", lambda e: e.scalar_tensor_tensor(out=D2, in0=NB, scalar=-2.0, in1=TB,
                                                     op0=ALU.mult, op1=ALU.add), (bt,), (bt,))
        P.op("dve", lambda e: e.tensor_single_scalar(out=PAST01[:], in_=D2, scalar=2.0, op=ALU.is_ge),
             (bt,), (bConst,))
        P.op("dve", lambda e: e.tensor_scalar(out=PASTM[:], in0=PAST01[:], scalar1=1e30, scalar2=-1e30,
                                              op0=ALU.mult, op1=ALU.add), (bConst,), (bConst,))
        TMPO = R2[:, 8576:8704].rearrange("p (t n) -> p t n", t=16)
        P.op("dve", lambda e: e.tensor_single_scalar(out=TMPO, in_=D2, scalar=0.0, op=ALU.is_ge), (bt,), (bt,))
        P.op("dve", lambda e: e.tensor_single_scalar(out=OWN01[:], in_=D2, scalar=1.0, op=ALU.is_le),
             (bt,), (bConst,))
        P.op("dve", lambda e: e.tensor_tensor(out=OWN01[:], in0=OWN01[:], in1=TMPO, op=ALU.mult),
             (bt, bConst), (bConst,))
        P.dma("sp", lambda e: e.dma_start(out=GT[:], in_=pre_gT.rearrange("l p c -> p l c")), (), (bConst,))
        P.fence()

    def phase_a(s, l):
        xsrc = x_in if l == LAYERS[0] else out
        for i in range(NT):
            k = i % 2
            P.dma("sp", lambda e, i=i, k=k: e.dma_start(out=XT[k], in_=xsrc[s, 128 * i:128 * i + 128, :]),
                  (bX[s][i],), (bXT[k],))
            ssc = SMALL[:, C_SS + k:C_SS + k + 1]
            rsc = SMALL[:, C_RSTD + k:C_RSTD + k + 1]
            P.op("act", lambda e, k=k, ssc=ssc: e.activation(out=JUNK, in_=XT[k], func=AF.Square, accum_out=ssc),
                 (bXT[k],), (bJ, bSS[k]))
            P.op("dve", lambda e, ssc=ssc, rsc=rsc: e.tensor_scalar(out=rsc, in0=ssc, scalar1=1.0 / D, scalar2=EPS,
                                                                     op0=ALU.mult, op1=ALU.add), (bSS[k],), (bSS[k],))
            P.op("act", lambda e, rsc=rsc: e.activation(out=rsc, in_=rsc, func=AF.Sqrt), (bSS[k],), (bSS[k],))
            P.op("dve", lambda e, rsc=rsc: e.reciprocal(out=rsc, in_=rsc), (bSS[k],), (bSS[k],))
            P.op("dve", lambda e, k=k, rsc=rsc: e.tensor_scalar(out=XN[k], in0=XT[k], scalar1=rsc, scalar2=None,
                                                                 op0=ALU.mult), (bXT[k], bSS[k]), (bXN[k],))
            for g in range(2):
                for j in range(8):
                    c = 8 * g + j
                    P.op("pe", lambda e, k=k, c=c, j=j: e.transpose(out=psT[0][:, j, :],
                                                                     in_=XN[k][:, 128 * c:128 * c + 128],
                                                                     identity=IDENT[:]),
                         (bXN[k], bConst), (bT[0],))
                gin = GT[:, l, 8 * g:8 * g + 8].unsqueeze(2).to_broadcast([128, 8, 128])
                P.op("dve", lambda e, g=g, i=i, gin=gin: e.tensor_tensor(
                    out=R1[:, 8 * g:8 * g + 8, 128 * i:128 * i + 128], in0=psT[0][:, :, :], in1=gin, op=ALU.mult),
                    (bT[0], bConst), tuple(bR1[8 * g:8 * g + 8]))

    def load_w_to(w_ap, row0, col0, ncols, dst_fn, dbufs):
        k = st["stg"]
        st["stg"] ^= 1
        src = w_ap[row0:row0 + 1024, col0:col0 + ncols].rearrange("(c p) n -> p c n", p=128)
        P.dma("sp", lambda e: e.dma_start(out=STG[k][:, :, 0:ncols], in_=src), (), (bSTG[k],))
        P.op("pool", lambda e: e.tensor_copy(out=dst_fn(), in_=STG[k][:, :, 0:ncols]), (bSTG[k],), dbufs)

    def load_w(w_ap, col0, ncols, dst_col=0, slot=None):
        if slot is None:
            slot = st["ws"]
            st["ws"] ^= 1
        for g in range(2):
            load_w_to(w_ap, 1024 * g, col0, ncols,
                      lambda g=g: WS[slot][:, 8 * g:8 * g + 8, dst_col:dst_col + ncols], (bWS[slot],))
        return slot

    def proj_tok(w_ap, col0, ncols, consume):
        slot = load_w(w_ap, col0, ncols)
        for i in range(NT):
            a = st["pa"]
            st["pa"] ^= 1
            for c in range(NC):
                P.op("pe", lambda e, a=a, c=c, i=i: e.matmul(psA[a][:, 0:ncols], lhsT=R1[:, c, 128 * i:128 * i + 128],
                                                             rhs=WS[slot][:, c, 0:ncols], start=(c == 0),
                                                             stop=(c == NC - 1)),
                     (bR1[c], bWS[slot]), (bA[a],))
            consume(i, psA[a][:, 0:ncols], bA[a])

    def proj_feat(slot, wcol, M, dst_ap_fn, dbuf, scale=None, prow=0):
        for tb in range(4):
            a = st["pa"]
            st["pa"] ^= 1
            for c in range(NC):
                P.op("pe", lambda e, a=a, c=c, tb=tb: e.matmul(psA[a][prow:prow + M, :],
                                                               lhsT=WS[slot][:, c, wcol:wcol + M],
                                                               rhs=R1[:, c, 512 * tb:512 * tb + 512],
                                                               start=(c == 0), stop=(c == NC - 1)),
                     (bR1[c], bWS[slot]), (bA[a],))
            if isinstance(scale, (list, tuple)):
                for hh, sc in enumerate(scale):
                    copy_op(evac_engine(), dst_ap_fn(tb)[64 * hh:64 * hh + 64, :], psA[a][64 * hh:64 * hh + 64, :],
                            (bA[a],), (dbuf,), sc)
            else:
                copy_op(evac_engine(), dst_ap_fn(tb), psA[a][prow:prow + M, :], (bA[a],), (dbuf,), scale)

    def proj_z(w_ap, zcol0):
        for j in range(8):
            def consume(i, pap, pbuf, j=j):
                k = (i + j) % 2
                P.op("act", lambda e, k=k, pap=pap: e.activation(out=SZS[k][:, 0:256], in_=pap, func=AF.Silu),
                     (pbuf,), (bSZS[k],))
                P.dma("sp", lambda e, k=k, i=i, j=j: e.dma_start(out=SZ[128 * i:128 * i + 128, 256 * j:256 * j + 256],
                                                                 in_=SZS[k][:, 0:256]), (bSZS[k],), (bSZd[i],))
            proj_tok(w_ap, zcol0 + 256 * j, 256, consume)

    def attention_head(qsrc, ksrc, krow, nk, vrhs_fn, dv, items_fn, slope, auxq, auxk, naux, finalize,
                       nmaps=1, qb_list=range(4), bq=None, bk=None, baux=None):
        for qb in qb_list:
            items = items_fn(qb)
            sinfo = {}

            def emit_s(n):
                m, kt, jq0, masks, jq1 = items[n]
                a = st["pa"]
                st["pa"] ^= 1
                sinfo[n] = a
                q0 = 512 * qb
                r0 = krow(m)
                pieces = []
                col = 128 * jq0
                for mk in masks:
                    pieces.append((col, col + 128, mk))
                    col += 128
                if col < 128 * jq1:
                    pieces.append((col, 128 * jq1, None))
                for (lo, hi, mk) in pieces:
                    P.op("pe", lambda e, a=a, m=m, kt=kt, lo=lo, hi=hi, r0=r0: e.matmul(
                        psA[a][:, lo:hi], lhsT=ksrc(m)[r0:r0 + nk, 128 * kt:128 * kt + 128],
                        rhs=qsrc(m)[r0:r0 + nk, q0 + lo:q0 + hi], start=True, stop=False),
                        (bq, bk), (bA[a],))
                    P.op("pe", lambda e, a=a, kt=kt, lo=lo, hi=hi, mk=mk: e.matmul(
                        psA[a][:, lo:hi], lhsT=auxk[0:naux, 128 * kt:128 * kt + 128],
                        rhs=auxq[0:naux, q0 + lo:q0 + hi], start=False, stop=(mk is None)),
                        (baux, bConst), (bA[a],))
                    if mk is not None:
                        P.op("pe", lambda e, a=a, lo=lo, hi=hi, mk=mk: e.matmul(
                            psA[a][:, lo:hi], lhsT=IDENT[:], rhs=mk[:], start=False, stop=True),
                            (bConst,), (bA[a],))

            def emit_rest(n):
                m, kt, jq0, masks, jq1 = items[n]
                a = sinfo[n]
                p = st["pt"]
                st["pt"] = (st["pt"] + 1) % 3
                lo = 128 * jq0
                hi = 128 * jq1
                sl = float(slope(m))
                P.op("act", lambda e, a=a, p=p, lo=lo, hi=hi, sl=sl: e.activation(
                    out=PT[p][:, lo:hi], in_=psA[a][:, lo:hi], func=AF.Exp, scale=sl),
                     (bA[a],), (bPT[p],))
                for jq in range(jq0, jq1):
                    tq = 4 * qb + jq
                    first = first_kt(m, tq) == kt
                    last = (kt == tq)
                    P.op("pe", lambda e, p=p, jq=jq, m=m, kt=kt, first=first, last=last: e.matmul(
                        psO[:, jq, 256 * m:256 * m + dv + 1], lhsT=PT[p][:, 128 * jq:128 * jq + 128],
                        rhs=vrhs_fn(kt), start=first, stop=last),
                        (bPT[p], bVP[kt]), (bO[jq][m],))

            first_kt = items_fn.first_kt
            if items:
                emit_s(0)
            for n in range(len(items)):
                if n + 1 < len(items):
                    emit_s(n + 1)
                emit_rest(n)
            for jq in range(4):
                finalize(qb, jq)

    def norm_tail(qb, jq, h, dv, obuf_ap, ob):
        pass

    def y_store(qb, h, width, k):
        P.dma("sp", lambda e: e.dma_start(
            out=YS[512 * qb:512 * qb + 512, width * h:width * h + width].rearrange("(j p) d -> p j d", p=128),
            in_=YB[k][:, :, 0:width]), (bYB[k],), (bYd[qb],))

    def layer_diff(s, l):
        w_in = W["diff_w_in"]
        lam_init = 0.8 - 0.6 * math.exp(-0.3 * l)
        slopes = alibi_slopes(16)
        scale = 64 ** -0.5
        P.dma("sp", lambda e: e.dma_start(out=LAMW[:].rearrange("p a b -> p (a b)"),
                                          in_=diff_l.rearrange("a b -> (a b)").partition_broadcast(128)),
              (), (bSM,))
        lw = SMALL[:, C_TMP:C_TMP + 2]
        P.op("dve", lambda e: e.tensor_tensor(out=LAMW[:, 0, :], in0=LAMW[:, 0, :], in1=LAMW[:, 1, :], op=ALU.mult),
             (bSM,), (bSM,))
        P.op("dve", lambda e: e.tensor_tensor(out=LAMW[:, 2, :], in0=LAMW[:, 2, :], in1=LAMW[:, 3, :], op=ALU.mult),
             (bSM,), (bSM,))
        P.op("dve", lambda e: e.reduce_sum(out=lw[:, 0:1], in_=LAMW[:, 0, :], axis=AX.X), (bSM,), (bSM,))
        P.op("dve", lambda e: e.reduce_sum(out=lw[:, 1:2], in_=LAMW[:, 2, :], axis=AX.X), (bSM,), (bSM,))
        P.op("act", lambda e: e.activation(out=lw, in_=lw, func=AF.Exp), (bSM,), (bSM,))
        lam = SMALL[:, C_LAM:C_LAM + 1]
        P.op("dve", lambda e: e.scalar_tensor_tensor(out=lam, in0=lw[:, 0:1], scalar=lam_init, in1=lw[:, 1:2],
                                                     op0=ALU.add, op1=ALU.subtract), (bSM,), (bSM,))
        P.dma("sp", lambda e: e.dma_start(out=SUBG[:], in_=diff_subln[0, :].partition_broadcast(128)), (), (bSM,))
        P.op("dve", lambda e: e.tensor_scalar_mul(out=SUBG[:], in0=SUBG[:], scalar1=1.0 - lam_init), (bSM,), (bSM,))

        P.op("dve", lambda e: e.memset(AUXQ[0][0:8, :], 0.0), (), (bAUXQ[0],))
        phase_a(s, l)
        P.fence()
        P.op("dve", lambda e: e.memset(R2b[:, 0:16 * 16 * 129], 1.0), (), tuple(bVP))
        for j in range(8):
            def consume(i, pap, pbuf, j=j):
                eng = evac_engine()
                copy_op(eng, VP[:, i, 2 * j:2 * j + 2, 0:128], pap.rearrange("p (h d) -> p h d", h=2),
                        (pbuf,), (bVP[i],))
            proj_tok(w_in, 4096 + 256 * j, 256, consume)
        proj_z(w_in, 6144)

        def items_fn(qb):
            its = []
            for m in range(2):
                for kt in range(4 * qb + 4):
                    if kt < 4 * qb:
                        its.append((m, kt, 0, [], 4))
                    else:
                        its.append((m, kt, kt - 4 * qb, [CM], 4))
            return its
        items_fn.first_kt = lambda m, tq: 0

        for h in range(16):
            slot = st["ws"]
            st["ws"] ^= 1
            load_w(w_in, 128 * h, 128, 0, slot)
            load_w(w_in, 2048 + 128 * h, 128, 128, slot)
            k = h % 2
            proj_feat(slot, 0, 128, lambda tb, k=k: QT[k][:, 512 * tb:512 * tb + 512], bQT[k],
                      scale=scale / slopes[h])
            proj_feat(slot, 128, 128, lambda tb, k=k: KT[k][:, 512 * tb:512 * tb + 512], bKT[k])

            def finalize(qb, jq, h=h):
                o = jq % 2
                r0 = SMALL[:, C_R0 + o:C_R0 + o + 1]
                r1 = SMALL[:, C_R1 + o:C_R1 + o + 1]
                ss = SMALL[:, C_SS2 + o:C_SS2 + o + 1]
                P.op("dve", lambda e: e.reciprocal(out=r0, in_=psO[:, jq, 128:129]), (bO[jq][0],), (bOW[o],))
                P.op("dve", lambda e: e.reciprocal(out=r1, in_=psO[:, jq, 256 + 128:256 + 129]), (bO[jq][1],),
                     (bOW[o],))
                P.op("dve", lambda e: e.tensor_tensor(out=r1, in0=r1, in1=lam, op=ALU.mult), (bOW[o], bSM), (bOW[o],))
                P.op("dve", lambda e: e.tensor_scalar(out=OW2[o][:], in0=psO[:, jq, 256:256 + 128], scalar1=r1,
                                                      scalar2=None, op0=ALU.mult), (bO[jq][1], bOW[o]), (bOW2[o],))
                P.op("dve", lambda e: e.scalar_tensor_tensor(out=OW[o][:], in0=psO[:, jq, 0:128], scalar=r0,
                                                             in1=OW2[o][:], op0=ALU.mult, op1=ALU.subtract),
                     (bO[jq][0], bOW2[o], bOW[o]), (bOW[o],))
                P.op("act", lambda e: e.activation(out=OW2[o][:], in_=OW[o][:], func=AF.Square, accum_out=ss),
                     (bOW[o],), (bOW2[o],))
                P.op("dve", lambda e: e.tensor_scalar(out=ss, in0=ss, scalar1=1.0 / 128, scalar2=EPS,
                                                      op0=ALU.mult, op1=ALU.add), (bOW2[o],), (bOW2[o],))
                P.op("act", lambda e: e.activation(out=ss, in_=ss, func=AF.Sqrt), (bOW2[o],), (bOW2[o],))
                P.op("dve", lambda e: e.reciprocal(out=ss, in_=ss), (bOW2[o],), (bOW2[o],))
                kb = (h * 4 + qb) % 2
                P.op("dve", lambda e: e.scalar_tensor_tensor(out=YB[kb][:, jq, :], in0=OW[o][:], scalar=ss,
                                                             in1=SUBG[:], op0=ALU.mult, op1=ALU.mult),
                     (bOW[o], bOW2[o], bSM), (bYB[kb],))
                if jq == 3:
                    y_store(qb, h, 128, kb)

            attention_head(lambda m, k=k: QT[k], lambda m, k=k: KT[k], lambda m: 64 * m, 64,
                           lambda kt, h=h: VP[:, kt, h, :], 128, items_fn, (lambda m, h=h: slopes[h]), AUXQ[0], AUXK, 12, finalize,
                           nmaps=2, bq=bQT[k], bk=bKT[k], baux=bAUXQ[0])
        P.fence()
        phase_d(s, l, W["diff_w_out"])

    def layer_s5(s, l):
        w_in = W["s5_w_in"]
        TWO_PI = 6.283185
        INV2PI = 1.0 / (2.0 * math.pi)
        UT = R2b[:, 0:NC * 2048].rearrange("p (c t) -> p c t", c=NC)
        bUT = [Buf(f"UT{c}") for c in range(NC)]
        slots = []
        for tl in (WS[0], WS[1]):
            v = tl[:, :, :].rearrange("p c n -> p (c n)").bitcast(F32)
            slots += [v[:, 512 * k:512 * k + 512] for k in range(4)]
        for tl in (STG[0], STG[1]):
            v = tl[:, :, :].rearrange("p c n -> p (c n)")
            slots += [v[:, 512 * k:512 * k + 512] for k in range(4)]
        bS = [Buf(f"slot{i}") for i in range(16)]
        IOTA_S, COST, SINT, A_sb, B_sb, T13, T2s, T4s, W_re, W_im, Z_re, Z_im = slots[0:12]
        PB = [slots[12].bitcast(BF16)[:, 0:512], slots[12].bitcast(BF16)[:, 512:1024],
              slots[13].bitcast(BF16)[:, 0:512], slots[13].bitcast(BF16)[:, 512:1024]]
        bPB = [Buf(), Buf(), Buf(), Buf()]
        YSf = [slots[14], slots[14]]
        NSINT = slots[15]
        TIi = slots[5].bitcast(I32)

        phase_a(s, l)
        P.fence()
        if DEBUG_STOP[0] == 5:
            phase_d(s, l, W["s5_w_out"], s5=True)
            return
        for c in range(NC):
            slot = st["ws"]
            st["ws"] ^= 1
            load_w(w_in, 128 * c, 128, 0, slot)
            load_w(w_in, 2048 + 128 * c, 128, 128, slot)
            proj_feat(slot, 0, 128, lambda tb, c=c: UT[:, c, 512 * tb:512 * tb + 512], bUT[c])
            for tb in range(4):
                a = st["pa"]
                st["pa"] ^= 1
                for ci in range(NC):
                    P.op("pe", lambda e, a=a, ci=ci, tb=tb, slot=slot: e.matmul(
                        psA[a][:, :], lhsT=WS[slot][:, ci, 128:256], rhs=R1[:, ci, 512 * tb:512 * tb + 512],
                        start=(ci == 0), stop=(ci == NC - 1)), (bR1[ci], bWS[slot]), (bA[a],))
                k = tb % 2
                P.op("act", lambda e, a=a, k=k: e.activation(out=SZS[k][:], in_=psA[a][:, :], func=AF.Silu),
                     (bA[a],), (bSZS[k],))
                P.dma("sp", lambda e, k=k, c=c, tb=tb: e.dma_start(out=ZT[c, :, 512 * tb:512 * tb + 512], in_=SZS[k][:]),
                      (bSZS[k],), (bSZd[c],))
        P.fence()
        if DEBUG_STOP[0] == 1:
            return
        R1f = R1[:, :, :].rearrange("p c t -> p (c t)")
        TB = [[R1f[:, 2048 * (2 * ri + v):2048 * (2 * ri + v) + 2048].rearrange("p (c n) -> p c n", c=NC)
               for v in range(2)] for ri in range(2)]
        CTre = R1f[:, 8192:16384].rearrange("p (g n) -> p g n", g=64)
        CTim = R1f[:, 16384:24576].rearrange("p (g n) -> p g n", g=64)
        scr = R1f[:, 24576:32768].bitcast(F32)
        Xs = [scr[:, 1024 * k:1024 * k + 1024] for k in range(4)]
        bt = Buf("s5tab")
        big = [slots[2 * k] for k in range(8)]
        P.dma("sp", lambda e: e.dma_start(out=X3[0], in_=s5p["s5_are_X"]), (), (bt,))
        P.dma("sp", lambda e: e.dma_start(out=X3[1], in_=s5p["s5_aim_X"]), (), (bt,))
        P.dma("sp", lambda e: e.dma_start(out=S5D[:, 0, :], in_=s5p["s5_dT"]), (), (bt,))
        P.dma("sp", lambda e: e.dma_start(out=S5D[:, 1, :], in_=s5p["s5_b_gluT"]), (), (bt,))
        LDT = SMALL[:, 64:80]
        P.dma("sp", lambda e: e.dma_start(out=LDT, in_=s5p["s5_ldt_X"]), (), (bt,))
        P.op("act", lambda e: e.activation(out=LDT, in_=LDT, func=AF.Exp), (bt,), (bt,))
        dtb = LDT.unsqueeze(2).to_broadcast([128, NC, 64])
        def pair(i):
            tl = (WS[0], WS[1], STG[0], STG[1])[i // 2]
            v = tl[:, :, :].rearrange("p c n -> p (c n)")
            if i < 4:
                v = v.bitcast(F32)
            return v[:, 1024 * (i % 2):1024 * (i % 2) + 1024]
        X = [pair(i) for i in range(4, 8)]
        X3 = [x.rearrange("p (c n) -> p c n", c=NC) for x in X]
        Y = Xs + [pair(i) for i in range(4)]
        Y3 = [y.rearrange("p (c n) -> p c n", c=NC) for y in Y]
        TT_ = lambda eng, o, a, b, op: P.op(eng, lambda e: e.tensor_tensor(out=o, in0=a, in1=b, op=op), (bt,), (bt,))
        TS_ = lambda eng, o, a, s1, s2, o0, o1=None: P.op(eng, lambda e: (
            e.tensor_scalar(out=o, in0=a, scalar1=s1, scalar2=s2, op0=o0, op1=o1) if o1 is not None else
            e.tensor_scalar(out=o, in0=a, scalar1=s1, scalar2=None, op0=o0)), (bt,), (bt,))

        def sincos_turns(tp, sin_o, cos_o, tmp_f, tmp_i):
            P.op("dve", lambda e: e.tensor_copy(out=tmp_i, in_=tp), (bt,), (bt,))
            TT_("dve", tmp_f, tp, tmp_i, ALU.subtract)
            P.op("act", lambda e: e.activation(out=sin_o, in_=tmp_f, func=AF.Sin, scale=TWO_PI), (bt,), (bt,))
            TS_("dve", tmp_f, tp, 0.25, None, ALU.add)
            P.op("dve", lambda e: e.tensor_copy(out=tmp_i, in_=tmp_f), (bt,), (bt,))
            TT_("dve", tmp_f, tmp_f, tmp_i, ALU.subtract)
            P.op("act", lambda e: e.activation(out=cos_o, in_=tmp_f, func=AF.Sin, scale=TWO_PI), (bt,), (bt,))

        LR, LI = X[0], X[1]
        TT_("dve", Y3[0], X3[0], dtb, ALU.mult)
        P.op("act", lambda e: e.activation(out=Y[0], in_=Y[0], func=AF.Exp), (bt,), (bt,))
        TT_("dve", Y3[1], X3[1], dtb, ALU.mult)
        TS_("dve", Y[1], Y[1], INV2PI, None, ALU.mult)
        sincos_turns(Y[1], Y[2], Y[3], Y[4], Y[5].bitcast(I32))
        TT_("dve", Y[3], Y[3], Y[0], ALU.mult)
        TT_("dve", Y[2], Y[2], Y[0], ALU.mult)
        TS_("dve", Y[3], Y[3], -1.0, None, ALU.add)
        TT_("dve", Y[0], LR, LR, ALU.mult)
        TT_("dve", Y[1], LI, LI, ALU.mult)
        TT_("dve", Y[0], Y[0], Y[1], ALU.add)
        P.op("dve", lambda e: e.reciprocal(out=Y[0], in_=Y[0]), (bt,), (bt,))
        TT_("dve", Y[4], Y[3], LR, ALU.mult)
        TT_("dve", Y[5], Y[2], LI, ALU.mult)
        TT_("dve", Y[4], Y[4], Y[5], ALU.add)
        TT_("dve", Y[4], Y[4], Y[0], ALU.mult)
        TT_("dve", Y[5], Y[2], LR, ALU.mult)
        TT_("dve", Y[1], Y[3], LI, ALU.mult)
        TT_("dve", Y[5], Y[5], Y[1], ALU.subtract)
        TT_("dve", Y[5], Y[5], Y[0], ALU.mult)
        P.dma("sp", lambda e: e.dma_start(out=X3[2], in_=s5p["s5_bre_X"]), (), (bt,))
        P.dma("sp", lambda e: e.dma_start(out=X3[3], in_=s5p["s5_bim_X"]), (), (bt,))
        TT_("dve", Y[0], Y[4], X[2], ALU.mult)
        TT_("dve", Y[1], Y[5], X[3], ALU.mult)
        TT_("dve", Y[0], Y[0], Y[1], ALU.subtract)
        TT_("dve", Y[2], Y[4], X[3], ALU.mult)
        TT_("dve", Y[3], Y[5], X[2], ALU.mult)
        TT_("dve", Y[2], Y[2], Y[3], ALU.add)
        PI_ = SMALL[:, 80:81].bitcast(I32)
        PJ_ = SMALL[:, 81:82].bitcast(I32)
        PK_ = SMALL[:, 82:83].bitcast(I32)
        P.op("pool", lambda e: e.iota(PI_, pattern=[[0, 1]], base=0, channel_multiplier=1), (bt,), (bt,))
        P.op("dve", lambda e: e.tensor_scalar(out=PJ_, in0=PI_, scalar1=5, scalar2=1, op0=ALU.logical_shift_right,
                                              op1=ALU.bitwise_and), (bt,), (bt,))
        P.op("dve", lambda e: e.tensor_scalar(out=PK_, in0=PI_, scalar1=4, scalar2=1, op0=ALU.logical_shift_right,
                                              op1=ALU.bitwise_and), (bt,), (bt,))
        FJ = SMALL[:, 84:85]
        FK = SMALL[:, 85:86]
        P.op("dve", lambda e: e.tensor_copy(out=FJ, in_=PJ_), (bt,), (bt,))
        P.op("dve", lambda e: e.tensor_copy(out=FK, in_=PK_), (bt,), (bt,))
        for v in range(2):
            for g2 in range(2):
                mc = MSK[:, 2 * v + g2:2 * v + g2 + 1]
                P.op("dve", lambda e, mc=mc, v=v: e.tensor_single_scalar(out=mc, in_=FJ, scalar=float(v), op=ALU.is_equal),
                     (bt,), (bt,))
                P.op("dve", lambda e, mc=mc, g2=g2: e.scalar_tensor_tensor(out=mc, in0=FK, scalar=float(g2), in1=mc,
                                                                          op0=ALU.is_equal, op1=ALU.mult), (bt,), (bt,))
                for ri, src in ((0, Y3[0]), (1, Y3[2])):
                    P.op("dve", lambda e, ri=ri, v=v, g2=g2, mc=mc, src=src: e.tensor_scalar(
                        out=TB[ri][v][:, :, 64 * g2:64 * g2 + 64], in0=src, scalar1=mc, scalar2=None, op0=ALU.mult),
                        (bt,), (bt,))
        THP, RHO, CR, SR, SRN, TMPa, TMPb, TMPc = [S5P[:, k, :] for k in range(8)]
        P.dma("sp", lambda e: e.dma_start(out=TMPa, in_=s5p["s5_ldt_Y"]), (), (bt,))
        P.dma("sp", lambda e: e.dma_start(out=RHO, in_=s5p["s5_are_Y"]), (), (bt,))
        P.dma("sp", lambda e: e.dma_start(out=THP, in_=s5p["s5_aim_Y"]), (), (bt,))
        P.op("act", lambda e: e.activation(out=TMPa, in_=TMPa, func=AF.Exp), (bt,), (bt,))
        TT_("dve", RHO, RHO, TMPa, ALU.mult)
        P.op("act", lambda e: e.activation(out=RHO, in_=RHO, func=AF.Exp), (bt,), (bt,))
        TT_("dve", THP, THP, TMPa, ALU.mult)
        TS_("dve", THP, THP, INV2PI, None, ALU.mult)
        TS_("dve", TMPa, THP, 512.0, None, ALU.mult)
        sincos_turns(TMPa, SR, CR, TMPb, TMPc.bitcast(I32))
        TS_("dve", SRN, SR, -1.0, None, ALU.mult)
        P.op("dve", lambda e: e.memset(R1f[:, 8192:24576], 0.0), (bt,), (bt,))
        CY = [X[2][:, :].rearrange("p (g c) -> p g c", c=16), X[3][:, :].rearrange("p (g c) -> p g c", c=16)]
        P.dma("sp", lambda e: e.dma_start(out=CY[0], in_=s5p["s5_cre_Y"]), (), (bt,))
        P.dma("sp", lambda e: e.dma_start(out=CY[1], in_=s5p["s5_cim_Y"]), (), (bt,))
        for ri, (CT_, sgn) in enumerate(((CTre, 1.0), (CTim, -1.0))):
            CT4 = CT_.rearrange("p (q j) n -> p q j n", j=4)
            CY4 = CY[ri].rearrange("p (q j) c -> p q j c", j=4)
            for j in range(4):
                for g2 in range(2):
                    P.op("dve", lambda e, CT4=CT4, CY4=CY4, j=j, g2=g2, sgn=sgn: e.tensor_scalar_mul(
                        out=CT4[64 * g2:64 * g2 + 64, :, j, 32 * j + 16 * g2:32 * j + 16 * g2 + 16],
                        in0=CY4[64 * g2:64 * g2 + 64, :, j, :], scalar1=sgn), (bt,), (bt,))
        P.fence()
        if DEBUG_STOP[0] == 2:
            return
        P.op("pool", lambda e: e.iota(IOTA_S, pattern=[[1, 512]], base=0, channel_multiplier=0,
                                      allow_small_or_imprecise_dtypes=True), (), (bS[0],))
        for cq in range(DEBUG_STOP[1]):
            for j in range(4):
                gp = 4 * cq + j
                hb = 64 * (j // 2)
                v = j % 2
                thc = THP[:, gp:gp + 1]
                P.op("dve", lambda e, thc=thc: e.tensor_scalar(out=A_sb, in0=IOTA_S, scalar1=thc, scalar2=None,
                                                               op0=ALU.mult), (bS[0], bt), (bS[3],))
                P.op("dve", lambda e: e.tensor_copy(out=TIi, in_=A_sb), (bS[3],), (bS[5],))
                P.op("dve", lambda e: e.tensor_tensor(out=B_sb, in0=A_sb, in1=TIi, op=ALU.subtract),
                     (bS[3], bS[5]), (bS[4],))
                P.op("act", lambda e: e.activation(out=SINT, in_=B_sb, func=AF.Sin, scale=TWO_PI), (bS[4],), (bS[2],))
                P.op("act", lambda e: e.activation(out=NSINT, in_=B_sb, func=AF.Sin, scale=-TWO_PI), (bS[4],), (bS[15],))
                P.op("dve", lambda e: e.tensor_scalar(out=A_sb, in0=A_sb, scalar1=0.25, scalar2=None, op0=ALU.add),
                     (bS[3],), (bS[3],))
                P.op("dve", lambda e: e.tensor_copy(out=TIi, in_=A_sb), (bS[3],), (bS[5],))
                P.op("dve", lambda e: e.tensor_tensor(out=B_sb, in0=A_sb, in1=TIi, op=ALU.subtract),
                     (bS[3], bS[5]), (bS[4],))
                P.op("act", lambda e: e.activation(out=COST, in_=B_sb, func=AF.Sin, scale=TWO_PI), (bS[4],), (bS[1],))
                rho_b = RHO[:, gp:gp + 1].to_broadcast([128, 512])
                for tb in range(0 if DEBUG_STOP[2] == 2 else 4):
                    blk = slice(512 * tb, 512 * tb + 512)
                    P.op("pe", lambda e, cq=cq, hb=hb, v=v, blk=blk: e.matmul(
                        psA[0][:, :], lhsT=TB[0][v][hb:hb + 64, cq, :], rhs=UT[hb:hb + 64, cq, blk],
                        start=True, stop=True), (bt, bUT[cq]), (bA[0],))
                    P.op("pe", lambda e, cq=cq, hb=hb, v=v, blk=blk: e.matmul(
                        psA[1][:, :], lhsT=TB[1][v][hb:hb + 64, cq, :], rhs=UT[hb:hb + 64, cq, blk],
                        start=True, stop=True), (bt, bUT[cq]), (bA[1],))
                    P.op("act", lambda e: e.copy(out=A_sb, in_=psA[0][:, :]), (bA[0],), (bS[3],))
                    P.op("act", lambda e: e.copy(out=B_sb, in_=psA[1][:, :]), (bA[1],), (bS[4],))
                    if DEBUG_STOP[2] == 5:
                        continue
                    P.op("pool", lambda e: e.tensor_tensor(out=T2s, in0=SINT, in1=B_sb, op=ALU.mult),
                         (bS[2], bS[4]), (bS[6],))
                    P.op("pool", lambda e: e.tensor_tensor(out=T4s, in0=SINT, in1=A_sb, op=ALU.mult),
                         (bS[2], bS[3]), (bS[7],))
                    if DEBUG_STOP[2] == 6:
                        continue
                    P.op("dve", lambda e: e.tensor_tensor(out=T13, in0=COST, in1=A_sb, op=ALU.mult),
                         (bS[1], bS[3]), (bS[5],))
                    P.op("dve", lambda e: e.tensor_tensor(out=W_re, in0=T13, in1=T2s, op=ALU.add),
                         (bS[5], bS[6]), (bS[8],))
                    P.op("dve", lambda e: e.tensor_tensor(out=T13, in0=COST, in1=B_sb, op=ALU.mult),
                         (bS[1], bS[4]), (bS[5],))
                    P.op("dve", lambda e: e.tensor_tensor(out=W_im, in0=T13, in1=T4s, op=ALU.subtract),
                         (bS[5], bS[7]), (bS[9],))
                    if DEBUG_STOP[2] == 3:
                        continue
                    ini_re = 0.0 if tb == 0 else CAR[:, 0:1]
                    ini_im = 0.0 if tb == 0 else CAR[:, 1:2]
                    P.op("dve", lambda e, ini_re=ini_re, rho_b=rho_b: e.tensor_tensor_scan(
                        out=Z_re, data0=rho_b, data1=W_re, initial=ini_re, op0=ALU.mult, op1=ALU.add),
                        (bS[8], bt, bSM), (bS[10],))
                    P.op("dve", lambda e, ini_im=ini_im, rho_b=rho_b: e.tensor_tensor_scan(
                        out=Z_im, data0=rho_b, data1=W_im, initial=ini_im, op0=ALU.mult, op1=ALU.add),
                        (bS[9], bt, bSM), (bS[11],))
                    if DEBUG_STOP[2] == 4:
                        continue
                    P.op("dve", lambda e: e.tensor_tensor(out=PB[0], in0=COST, in1=Z_re, op=ALU.mult),
                         (bS[1], bS[10]), (bPB[0],))
                    P.op("dve", lambda e: e.tensor_tensor(out=PB[2], in0=COST, in1=Z_im, op=ALU.mult),
                         (bS[1], bS[11]), (bPB[2],))
                    P.op("pool", lambda e: e.tensor_tensor(out=PB[1], in0=NSINT, in1=Z_im, op=ALU.mult),
                         (bS[15], bS[11]), (bPB[1],))
                    P.op("pool", lambda e: e.tensor_tensor(out=PB[3], in0=SINT, in1=Z_re, op=ALU.mult),
                         (bS[2], bS[10]), (bPB[3],))
                    for q, (CT_, pb) in enumerate(((CTre, 0), (CTre, 1), (CTim, 2), (CTim, 3))):
                        P.op("pe", lambda e, CT_=CT_, pb=pb, gp=gp, tb=tb, j=j, q=q: e.matmul(
                            psO[:, tb, :], lhsT=CT_[:, gp, :], rhs=PB[pb], start=(j == 0 and q == 0),
                            stop=(j == 3 and q == 3)), (bt, bPB[pb]), (bO[tb][0],))
                    if tb < 3:
                        crc, src, srnc = CR[:, gp:gp + 1], SR[:, gp:gp + 1], SRN[:, gp:gp + 1]
                        zr, zi = Z_re[:, 511:512], Z_im[:, 511:512]
                        P.op("dve", lambda e, zr=zr, crc=crc: e.tensor_scalar(out=CAR[:, 2:3], in0=zr, scalar1=crc,
                                                                              scalar2=None, op0=ALU.mult),
                             (bS[10], bt), (bSM,))
                        P.op("dve", lambda e, zr=zr, src=src: e.tensor_scalar(out=CAR[:, 3:4], in0=zr, scalar1=src,
                                                                              scalar2=None, op0=ALU.mult),
                             (bS[10], bt), (bSM,))
                        P.op("dve", lambda e, zi=zi, srnc=srnc: e.scalar_tensor_tensor(
                            out=CAR[:, 0:1], in0=zi, scalar=srnc, in1=CAR[:, 2:3], op0=ALU.mult, op1=ALU.add),
                            (bS[11], bt, bSM), (bSM,))
                        P.op("dve", lambda e, zi=zi, crc=crc: e.scalar_tensor_tensor(
                            out=CAR[:, 1:2], in0=zi, scalar=crc, in1=CAR[:, 3:4], op0=ALU.mult, op1=ALU.add),
                            (bS[11], bt, bSM), (bSM,))
            for tb in range(0 if DEBUG_STOP[2] in (2, 3, 4, 5, 6) else 4):
                k = tb % 2
                blk = slice(512 * tb, 512 * tb + 512)
                P.op("dve", lambda e, cq=cq, tb=tb, k=k, blk=blk: e.scalar_tensor_tensor(
                    out=YSf[k], in0=UT[:, cq, blk], scalar=S5D[:, 0, cq:cq + 1], in1=psO[:, tb, :],
                    op0=ALU.mult, op1=ALU.add), (bUT[cq], bO[tb][0], bt), (bS[14],))
                P.op("act", lambda e, cq=cq, k=k, blk=blk: e.activation(out=UT[:, cq, blk], in_=YSf[k],
                                                                        func=(AF.Copy if DEBUG_STOP[2] == 1 else AF.Gelu_apprx_tanh)),
                     (bS[14],), (bUT[cq],))
        P.fence()
        if DEBUG_STOP[0] == 3:
            return
        for co in range(NC):
            slot = st["ws"]
            st["ws"] ^= 1
            load_w(W["s5_w_glu"], 128 * co, 128, 0, slot)
            for tb in range(4):
                blk = slice(512 * tb, 512 * tb + 512)
                a = st["pa"]
                st["pa"] ^= 1
                k = tb % 2
                P.dma("sp", lambda e, k=k, co=co, blk=blk: e.dma_start(out=SZS[k][:], in_=ZT[co, :, blk]),
                      (bSZd[co],), (bSZS[k],))
                for ci in range(NC):
                    P.op("pe", lambda e, a=a, ci=ci, blk=blk, slot=slot: e.matmul(
                        psA[a][:, :], lhsT=WS[slot][:, ci, 0:128], rhs=UT[:, ci, blk],
                        start=(ci == 0), stop=(ci == NC - 1)), (bUT[ci], bWS[slot]), (bA[a],))
                o = tb % 2
                P.op("act", lambda e, a=a, o=o, co=co: e.activation(out=OWG[o][:], in_=psA[a][:, :], func=AF.Sigmoid,
                                                                    bias=S5D[:, 1, co:co + 1]),
                     (bA[a], bt), (bOWG[o],))
                P.op("dve", lambda e, o=o, co=co, blk=blk: e.tensor_tensor(out=OWG[o][:], in0=OWG[o][:], in1=UT[:, co, blk],
                                                                           op=ALU.mult), (bOWG[o], bUT[co]), (bOWG[o],))
                P.op("dve", lambda e, o=o, co=co, blk=blk, k=k: e.tensor_tensor(out=R1[:, co, blk], in0=OWG[o][:],
                                                                                in1=SZS[k][:], op=ALU.mult),
                     (bOWG[o], bSZS[k]), (bR1[co],))
        P.fence()
        if DEBUG_STOP[0] == 4:
            return
        phase_d(s, l, W["s5_w_out"], s5=True)


    def layer_swa(s, l):
        w_in = W["swa_w_in"]
        slopes = alibi_slopes(32)
        scale = 64 ** -0.5
        P.dma("sp", lambda e: e.dma_start(out=ESINK[:], in_=swa_sinks[0, :].partition_broadcast(128)), (), (bSM,))
        P.op("act", lambda e: e.activation(out=ESINK[:], in_=ESINK[:], func=AF.Exp), (bSM,), (bSM,))
        P.op("dve", lambda e: e.memset(AUXQ[0][0:8, :], 0.0), (), (bAUXQ[0],))
        phase_a(s, l)
        P.fence()
        P.op("dve", lambda e: e.memset(R2b[:, 0:16 * 4 * 65], 1.0), (), tuple(bVP))

        def consume_v(i, pap, pbuf):
            copy_op(evac_engine(), VPs[:, i, :, 0:64], pap.rearrange("p (h d) -> p h d", h=4), (pbuf,), (bVP[i],))
        proj_tok(w_in, 2304, 256, consume_v)
        proj_z(w_in, 2560)

        def items_fn(qb):
            its = []
            for m in range(2):
                if qb > 0:
                    its.append((m, 4 * qb - 1, 0, [CM2], 1))
                for j in range(4):
                    its.append((m, 4 * qb + j, j, [CM, CM2] if j < 3 else [CM], min(j + 2, 4)))
            return its
        items_fn.first_kt = lambda m, tq: max(tq - 1, 0)

        for kv in range(4):
            slot = st["ws"]
            st["ws"] ^= 1
            load_w(w_in, 2048 + 64 * kv, 64, 0, slot)
            proj_feat(slot, 0, 64, lambda tb: KT[0][0:64, 512 * tb:512 * tb + 512], bKT[0], prow=0)
            proj_feat(slot, 0, 64, lambda tb: KT[0][64:128, 512 * tb:512 * tb + 512], bKT[0], prow=64)
            for c in range(4 * kv, 4 * kv + 4):
                slot = st["ws"]
                st["ws"] ^= 1
                load_w(w_in, 128 * c, 128, 0, slot)
                proj_feat(slot, 0, 128, lambda tb: QT[0][:, 512 * tb:512 * tb + 512], bQT[0],
                          scale=[scale / slopes[2 * c], scale / slopes[2 * c + 1]])

                def finalize(qb, jq, c=c):
                    kb = (c * 4 + qb) % 2
                    for m in range(2):
                        o = m
                        r0 = SMALL[:, C_R0 + o:C_R0 + o + 1]
                        hq = 2 * c + m
                        P.op("dve", lambda e, m=m, r0=r0, hq=hq: e.tensor_tensor(
                            out=r0, in0=psO[:, jq, 256 * m + 64:256 * m + 65], in1=ESINK[:, hq:hq + 1], op=ALU.add),
                            (bO[jq][0], bSM), (bOW[o],))
                        P.op("dve", lambda e, r0=r0: e.reciprocal(out=r0, in_=r0), (bOW[o],), (bOW[o],))
                        P.op("dve", lambda e, m=m, r0=r0: e.tensor_scalar(
                            out=YB[kb][:, jq, 64 * m:64 * m + 64], in0=psO[:, jq, 256 * m:256 * m + 64], scalar1=r0,
                            scalar2=None, op0=ALU.mult), (bO[jq][0], bOW[o]), (bYB[kb],))
                    if jq == 3:
                        y_store(qb, c, 128, kb)

                attention_head(lambda m: QT[0], lambda m: KT[0], lambda m: 64 * m, 64,
                               lambda kt, kv=kv: VPs[:, kt, kv, :], 64, items_fn,
                               (lambda m, c=c: slopes[2 * c + m]), AUXQ[0], AUXK, 12, finalize,
                               nmaps=2, bq=bQT[0], bk=bKT[0], baux=bAUXQ[0])
        P.fence()
        phase_d(s, l, W["swa_w_out"])

    def layer_moba(s, l):
        w_in = W["moba_w_in"]
        slopes = alibi_slopes(16)
        scale = 128 ** -0.5
        phase_a(s, l)
        P.fence()
        P.op("dve", lambda e: e.memset(R2b[:, 0:16 * 16 * 129], 1.0), (), tuple(bVP))
        for j in range(8):
            def consume(i, pap, pbuf, j=j):
                copy_op(evac_engine(), VP[:, i, 2 * j:2 * j + 2, 0:128], pap.rearrange("p (h d) -> p h d", h=2),
                        (pbuf,), (bVP[i],))
            proj_tok(w_in, 4096 + 256 * j, 256, consume)
        proj_z(w_in, 6144)

        def items_fn(qb):
            its = []
            for kt in range(4 * qb + 4):
                if kt < 4 * qb:
                    its.append((0, kt, 0, [], 4))
                else:
                    its.append((0, kt, kt - 4 * qb, [CM], 4))
            return its
        items_fn.first_kt = lambda m, tq: 0

        for h in range(16):
            slot = st["ws"]
            st["ws"] ^= 1
            load_w(w_in, 128 * h, 128, 0, slot)
            load_w(w_in, 2048 + 128 * h, 128, 128, slot)
            proj_feat(slot, 0, 128, lambda tb: QT[0][:, 512 * tb:512 * tb + 512], bQT[0], scale=scale / slopes[h])
            proj_feat(slot, 128, 128, lambda tb: KT[0][:, 512 * tb:512 * tb + 512], bKT[0])
            P.op("dve", lambda e: e.reduce_sum(out=KMEAN[:], in_=KT[0][:, :].rearrange("p (n k) -> p n k", n=8),
                                               axis=AX.X), (bKT[0],), (bGate,))
            P.op("dve", lambda e: e.tensor_copy(out=KMEANB[:], in_=KMEAN[:]), (bGate,), (bGate,))
            for t in range(16):
                P.op("pe", lambda e, t=t: e.matmul(psG[0][:, 8 * t:8 * t + 8], lhsT=QT[0][:, 128 * t:128 * t + 128],
                                                   rhs=KMEANB[:], start=True, stop=True), (bQT[0], bGate), (bG[0],))
            P.op("dve", lambda e: e.tensor_tensor(out=GATE[:], in0=psG[0][:, 0:128].rearrange("p (t n) -> p t n", t=16),
                                                  in1=PASTM[:], op=ALU.add), (bG[0], bConst), (bGate,))
            for t in range(16):
                P.op("dve", lambda e, t=t: e.max(out=TOP8[:, t, :], in_=GATE[:, t, :]), (bGate,), (bMaskv,))
            P.op("dve", lambda e: e.tensor_tensor(out=GATE2[:], in0=GATE[:],
                                                  in1=TOP8[:, :, 2:3].to_broadcast([128, 16, 8]), op=ALU.is_ge),
                 (bGate, bMaskv), (bMaskv,))
            P.op("dve", lambda e: e.tensor_tensor(out=GATE2[:], in0=GATE2[:], in1=PAST01[:], op=ALU.mult),
                 (bMaskv, bConst), (bMaskv,))
            P.op("dve", lambda e: e.tensor_tensor(out=GATE2[:], in0=GATE2[:], in1=OWN01[:], op=ALU.add),
                 (bMaskv, bConst), (bMaskv,))
            P.op("dve", lambda e: e.tensor_scalar(out=MASKV[:], in0=GATE2[:], scalar1=BIG, scalar2=-BIG,
                                                  op0=ALU.mult, op1=ALU.add), (bMaskv,), (bMaskv,))
            for g in range(2):
                for j in range(8):
                    t = 8 * g + j
                    P.op("pe", lambda e, t=t, j=j: e.transpose(out=psT[0][0:8, j, :], in_=MASKV[:, t, :],
                                                               identity=IDENT[:]), (bMaskv, bConst), (bT[0],))
                copy_op(evac_engine(), AUXQ[0][0:8, 1024 * g:1024 * g + 1024].rearrange("p (j q) -> p j q", j=8),
                        psT[0][0:8, :, :], (bT[0],), (bAUXQ[0],))

            def finalize(qb, jq, h=h):
                o = jq % 2
                kb = (h * 4 + qb) % 2
                r0 = SMALL[:, C_R0 + o:C_R0 + o + 1]
                P.op("dve", lambda e: e.reciprocal(out=r0, in_=psO[:, jq, 128:129]), (bO[jq][0],), (bOW[o],))
                P.op("dve", lambda e: e.tensor_scalar(out=YB[kb][:, jq, :], in0=psO[:, jq, 0:128], scalar1=r0,
                                                      scalar2=None, op0=ALU.mult), (bO[jq][0], bOW[o]), (bYB[kb],))
                if jq == 3:
                    y_store(qb, h, 128, kb)

            attention_head(lambda m: QT[0], lambda m: KT[0], lambda m: 0, 128,
                           lambda kt, h=h: VP[:, kt, h, :], 128, items_fn, (lambda m, h=h: slopes[h]),
                           AUXQ[0], AUXK, 12, finalize, nmaps=1, bq=bQT[0], bk=bKT[0], baux=bAUXQ[0])
        P.fence()
        phase_d(s, l, W["moba_w_out"])

    def phase_d(s, l, w_out, s5=False):
        xsrc = x_in if l == LAYERS[0] else out
        if s5:
            Wd = R2b[:, 0:NC * 2048].rearrange("p (c t) -> p c t", c=NC)
            bWd = [Buf(f"wd{c}") for c in range(NC)]
            XTl = [STG[k][:, :, :].rearrange("p c n -> p (c n)") for k in range(2)]
            PGl = WS[0][:, :, :].rearrange("p c n -> p (c n)").bitcast(F32)
            TTl = WS[1][:, :, :].rearrange("p c n -> p (c n)").bitcast(F32)
            JUNKl = QT[0][:, :]
        else:
            Wd, bWd = R1, bR1
            XTl, PGl, TTl, JUNKl = XT, PG, TT, JUNK
        for nb in range(8):
            for g in range(2):
                load_w_to(w_out, 1024 * g, 256 * nb, 256,
                          lambda g=g, nb=nb: Wd[:, 8 * g:8 * g + 8, 256 * nb:256 * nb + 256],
                          tuple(bWd[8 * g:8 * g + 8]))
        if s5:
            P.fence()
            P.dma("sp", lambda e: e.dma_start(out=XTl[0], in_=post_g[l, :].partition_broadcast(128)), (), (bXT[0],))
            P.op("dve", lambda e: e.tensor_copy(out=PGl, in_=XTl[0]), (bXT[0],), (bPG,))
        else:
            P.dma("sp", lambda e: e.dma_start(out=PGl, in_=post_g[l, :].partition_broadcast(128)), (), (bPG,))
        for i in range(NT):
            k = i % 2
            P.dma("sp", lambda e, i=i, k=k: e.dma_start(out=XTl[k], in_=xsrc[s, 128 * i:128 * i + 128, :]),
                  (bX[s][i],), (bXT[k],))
            if not s5:
                P.dma("sp", lambda e, i=i, k=k: e.dma_start(out=Yt[k], in_=YS[128 * i:128 * i + 128, :]),
                      (bYd[i // 4],), (bYt[k],))
                P.dma("sp", lambda e, i=i, k=k: e.dma_start(out=SZt[k], in_=SZ[128 * i:128 * i + 128, :]),
                      (bSZd[i],), (bSZt[k],))
                P.op("dve", lambda e, k=k: e.tensor_tensor(out=YG, in0=Yt[k], in1=SZt[k], op=ALU.mult),
                     (bYt[k], bSZt[k]), (bYG,))
                for g in range(2):
                    for j in range(8):
                        c = 8 * g + j
                        P.op("pe", lambda e, c=c, j=j: e.transpose(out=psT[0][:, j, :], in_=YG[:, 128 * c:128 * c + 128],
                                                                   identity=IDENT[:]), (bYG, bConst), (bT[0],))
                    copy_op(evac_engine(), YGTv[:, 8 * g:8 * g + 8, :], psT[0][:, :, :], (bT[0],), (bYGT,))
            for nb in range(4):
                for c in range(NC):
                    if s5:
                        P.op("pe", lambda e, nb=nb, c=c, i=i: e.matmul(
                            psO[:, nb, :], lhsT=R1[:, c, 128 * i:128 * i + 128], rhs=Wd[:, c, 512 * nb:512 * nb + 512],
                            start=(c == 0), stop=(c == NC - 1)), (bR1[c], bWd[c]), (bOall,))
                    else:
                        P.op("pe", lambda e, nb=nb, c=c: e.matmul(psO[:, nb, :], lhsT=YGTv[:, c, :],
                                                                  rhs=Wd[:, c, 512 * nb:512 * nb + 512],
                                                                  start=(c == 0), stop=(c == NC - 1)),
                             (bYGT, bWd[c]), (bOall,))
            ss = SMALL[:, C_SS2 + 4 + k:C_SS2 + 5 + k]
            P.op("act", lambda e, ss=ss: e.activation(out=JUNKl.rearrange("p (a b) -> p a b", a=4), in_=psO[:, :, :],
                                                      func=AF.Square, accum_out=ss), (bOall,), (bJ, bSS[k]))
            P.op("dve", lambda e, ss=ss: e.tensor_scalar(out=ss, in0=ss, scalar1=1.0 / D, scalar2=EPS,
                                                         op0=ALU.mult, op1=ALU.add), (bSS[k],), (bSS[k],))
            P.op("act", lambda e, ss=ss: e.activation(out=ss, in_=ss, func=AF.Sqrt), (bSS[k],), (bSS[k],))
            P.op("dve", lambda e, ss=ss: e.reciprocal(out=ss, in_=ss), (bSS[k],), (bSS[k],))
            P.op("dve", lambda e, ss=ss: e.scalar_tensor_tensor(out=TTl.rearrange("p (a b) -> p a b", a=4),
                                                                in0=psO[:, :, :], scalar=ss,
                                                                in1=PGl.rearrange("p (a b) -> p a b", a=4),
                                                                op0=ALU.mult, op1=ALU.mult),
                 (bOall, bSS[k], bPG), (bTT,))
            P.op("pool", lambda e, k=k: e.tensor_tensor(out=XTl[k], in0=XTl[k], in1=TTl, op=ALU.add),
                 (bXT[k], bTT), (bXT[k],))
            P.dma("sp", lambda e, i=i, k=k: e.dma_start(out=out[s, 128 * i:128 * i + 128, :], in_=XTl[k]),
                  (bXT[k],), (bX[s][i],))
        P.fence()

    setup()
    for s in range(NSEQ):
        for l in LAYERS:
            if l == 0:
                layer_s5(s, l)
            elif l == 1:
                layer_diff(s, l)
            elif l == 2:
                layer_moba(s, l)
            elif l == 3:
                layer_swa(s, l)
            else:
                raise NotImplementedError(l)

    sem_keys = list(ENG) + [("d", d) for d in range(P.NDMA)]
    sems = {}
    for kx in sem_keys:
        nm = kx if isinstance(kx, str) else f"dma{kx[1]}"
        sems[kx] = es.enter_context(nc.semaphore("sem_" + nm))
    with nc.Block() as block:
        P.emit(nc, block, sems)
    es.close()
    return nc


def host_inputs(inp, lo, hi):
    f = np.ascontiguousarray
    m = {"x": f(inp["x"][lo:hi])}
    m["pre_gT"] = f(inp["pre_norm"].reshape(4, NC, 128).transpose(0, 2, 1))
    m["post_norm"] = f(inp["post_norm"])
    for k in ("s5_w_in", "s5_w_glu", "s5_w_out", "diff_w_in", "diff_w_out", "moba_w_in", "moba_w_out",
              "swa_w_in", "swa_w_out"):
        m[k] = f(inp[k][0])
    m["diff_l"] = f(np.stack([inp["diff_lq1"][0], inp["diff_lk1"][0], inp["diff_lq2"][0], inp["diff_lk2"][0]]))
    m["diff_subln"] = f(inp["diff_subln"])
    m["swa_sinks"] = f(inp["swa_sinks"])
    a_re, a_im, ldt = inp["s5_a_re"][0], inp["s5_a_im"][0], inp["s5_log_dt"][0]
    b_re, b_im, c_re, c_im = inp["s5_b_re"][0], inp["s5_b_im"][0], inp["s5_c_re"][0], inp["s5_c_im"][0]

    def xlay(a):
        return f(np.broadcast_to(a.reshape(16, 8, 1, 64), (16, 8, 16, 64)).transpose(1, 2, 0, 3).reshape(128, 16, 64))
    m["s5_are_X"] = xlay(a_re)
    m["s5_aim_X"] = xlay(a_im)
    m["s5_ldt_X"] = f(np.broadcast_to(ldt.reshape(16, 8, 1), (16, 8, 16)).transpose(1, 2, 0).reshape(128, 16))
    m["s5_bre_X"] = f(b_re.reshape(16, 8, 64, 16).transpose(1, 3, 0, 2).reshape(128, 16, 64))
    m["s5_bim_X"] = f(b_im.reshape(16, 8, 64, 16).transpose(1, 3, 0, 2).reshape(128, 16, 64))
    m["s5_are_Y"] = f(a_re.reshape(64, 2, 64).transpose(1, 2, 0).reshape(128, 64))
    m["s5_aim_Y"] = f(a_im.reshape(64, 2, 64).transpose(1, 2, 0).reshape(128, 64))
    m["s5_ldt_Y"] = f(np.broadcast_to(ldt.reshape(64, 2, 1), (64, 2, 64)).transpose(1, 2, 0).reshape(128, 64))
    m["s5_cre_Y"] = f(c_re.reshape(64, 2, 16, 64).transpose(1, 3, 0, 2).reshape(128, 64, 16))
    m["s5_cim_Y"] = f(c_im.reshape(64, 2, 16, 64).transpose(1, 3, 0, 2).reshape(128, 64, 16))
    m["s5_dT"] = f(inp["s5_d"][0].reshape(NC, 128).T)
    m["s5_b_gluT"] = f(inp["s5_b_glu"][0].reshape(NC, 128).T)
    return {k: np.asarray(v, dtype=np.float32) for k, v in m.items()}


_CACHE = {}


def kernel(**inputs):
    inputs = {k: np.asarray(v) for k, v in inputs.items()}
    n = 8
    per = inputs["x"].shape[0] // n
    key = ("full", per)
    if key not in _CACHE:
        _CACHE[key] = build_program(per, [0, 1, 2, 3])
    nc = _CACHE[key]
    in_maps = [host_inputs(inputs, c * per, (c + 1) * per) for c in range(n)]
    res = run_bass_kernel_spmd(nc, in_maps, core_ids=list(range(n)))
    return np.concatenate([np.asarray(r["out"], dtype=np.float32) for r in res.results], axis=0)
```

```python
import math
from contextlib import ExitStack

import numpy as np
import concourse.bass as bass
import concourse.mybir as mybir
from concourse.bass_utils import run_bass_kernel_spmd

F32 = mybir.dt.float32
BF16 = mybir.dt.bfloat16
I32 = mybir.dt.int32
AF = mybir.ActivationFunctionType
ALU = mybir.AluOpType
AX = mybir.AxisListType

D = 2048
S = 2048
NT = 16
NC = 16
EPS = 1e-6
BIG = 60000.0
ENG = ("pe", "act", "dve", "pool", "sp")
DEBUG_STOP = [0, 16, 0]


class Buf:
    __slots__ = ("w", "r", "name")

    def __init__(self, name=""):
        self.w = None
        self.r = {}
        self.name = name


class Prog:
    NDMA = 24

    def __init__(self):
        self.ops = []
        self.cnt = {e: 0 for e in ENG}
        self.waited = {e: {} for e in ENG}
        self.dcnt = [0] * self.NDMA
        self.drr = 0
        self.fence_ev = {}

    def fence(self):
        ev = {e: self.cnt[e] for e in ENG if self.cnt[e]}
        for d in range(self.NDMA):
            if self.dcnt[d]:
                ev[("d", d)] = 16 * self.dcnt[d]
        self.fence_ev = ev

    def _deps(self, eng, reads, writes, extra=()):
        need = {}

        def add(ev):
            if ev is None:
                return
            k, v = ev
            if need.get(k, 0) < v:
                need[k] = v

        for b in reads:
            add(b.w)
        for b in writes:
            add(b.w)
            for k, v in b.r.items():
                add((k, v))
        for ev in extra:
            add(ev)
        for k, v in self.fence_ev.items():
            add((k, v))
        wl = []
        for k, v in need.items():
            if k == "pe" and eng == "pe":
                continue
            if self.waited[eng].get(k, 0) >= v:
                continue
            self.waited[eng][k] = v
            wl.append((k, v))
        return wl

    def op(self, eng, fn, reads=(), writes=()):
        wl = self._deps(eng, reads, writes)
        self.cnt[eng] += 1
        ev = (eng, self.cnt[eng])
        self.ops.append((eng, fn, wl, ev, 1))
        for b in reads:
            if b.r.get(eng, 0) < ev[1]:
                b.r[eng] = ev[1]
        for b in writes:
            b.w = ev
            b.r = {}
        return ev

    def dma(self, eng, fn, reads=(), writes=()):
        d = self.drr
        self.drr = (self.drr + 1) % self.NDMA
        key = ("d", d)
        extra = [(key, 16 * self.dcnt[d])] if self.dcnt[d] else []
        wl = self._deps(eng, reads, writes, extra)
        self.dcnt[d] += 1
        ev = (key, 16 * self.dcnt[d])
        self.ops.append((eng, fn, wl, ev, 16))
        for b in reads:
            b.r[key] = ev[1]
        for b in writes:
            b.w = ev
            b.r = {}
        return ev

    def emit(self, nc, block, sems):
        engmap = {"pe": block.tensor, "act": block.scalar, "dve": block.vector,
                  "pool": block.gpsimd, "sp": block.sync}
        for en in ENG:
            ops = [o for o in self.ops if o[0] == en]
            final = []
            if en == "sp":
                for d in range(self.NDMA):
                    if self.dcnt[d]:
                        final.append((("d", d), 16 * self.dcnt[d]))
                for e2 in ENG:
                    if e2 != "sp" and self.cnt[e2]:
                        final.append((e2, self.cnt[e2]))

            def body(e, ops=ops, final=final):
                for (_, fn, wl, ev, amt) in ops:
                    for k, v in wl:
                        e.wait_ge(sems[k], v)
                    ins = fn(e)
                    ins.then_inc(sems[ev[0]], amt)
                for k, v in final:
                    e.wait_ge(sems[k], v)

            engmap[en](body)


def alibi_slopes(n):
    return [2.0 ** (-8.0 * (h + 1) / n) for h in range(n)]


def build_program(NSEQ, LAYERS):
    nc = bass.Bass("TRN2", target_bir_lowering=False)
    P = Prog()

    def din(name, shape):
        return nc.dram_tensor(name, list(shape), F32, kind="ExternalInput").ap()

    x_in = din("x", [NSEQ, S, D])
    out = nc.dram_tensor("out", [NSEQ, S, D], F32, kind="ExternalOutput").ap()
    pre_gT = din("pre_gT", [4, 128, NC])
    post_g = din("post_norm", [4, D])
    W = {}
    W["s5_w_in"] = din("s5_w_in", [D, 2 * D])
    W["s5_w_glu"] = din("s5_w_glu", [D, D])
    W["s5_w_out"] = din("s5_w_out", [D, D])
    W["diff_w_in"] = din("diff_w_in", [D, 4 * D])
    W["diff_w_out"] = din("diff_w_out", [D, D])
    W["moba_w_in"] = din("moba_w_in", [D, 4 * D])
    W["moba_w_out"] = din("moba_w_out", [D, D])
    W["swa_w_in"] = din("swa_w_in", [D, 4608])
    W["swa_w_out"] = din("swa_w_out", [D, D])
    diff_l = din("diff_l", [4, 64])
    diff_subln = din("diff_subln", [1, 128])
    swa_sinks = din("swa_sinks", [1, 32])
    s5p = {}
    for nm, shp in (("s5_are_X", [128, 16, 64]), ("s5_aim_X", [128, 16, 64]), ("s5_ldt_X", [128, 16]),
                    ("s5_bre_X", [128, 16, 64]), ("s5_bim_X", [128, 16, 64]),
                    ("s5_are_Y", [128, 64]), ("s5_aim_Y", [128, 64]), ("s5_ldt_Y", [128, 64]),
                    ("s5_cre_Y", [128, 64, 16]), ("s5_cim_Y", [128, 64, 16]),
                    ("s5_dT", [128, NC]), ("s5_b_gluT", [128, NC])):
        s5p[nm] = din(nm, shp)

    SZ = nc.dram_tensor("scr_sz", [S, D], BF16).ap()
    YS = nc.dram_tensor("scr_y", [S, D], BF16).ap()
    ZT = nc.dram_tensor("scr_zt", [NC, 128, S], BF16).ap()

    es = ExitStack()

    def sb(name, shape, dt):
        return es.enter_context(nc.sbuf_tensor(name, list(shape), dt))

    def ps(name, shape, dt):
        return es.enter_context(nc.psum_tensor(name, list(shape), dt))

    R1 = sb("R1", [128, NC, 2048], BF16)
    R2 = sb("R2", [128, 16640], F32)
    WS = [sb(f"WS{i}", [128, NC, 256], BF16) for i in range(2)]
    STG = [sb(f"STG{i}", [128, 8, 256], F32) for i in range(2)]
    QT = [sb("QT0", [128, 2048], BF16)] * 2
    KT = [sb("KT0", [128, 2048], BF16)] * 2
    AUXQ = [sb("AUXQ0", [12, 2048], BF16)] * 2
    AUXK = sb("AUXK", [12, 2048], BF16)
    PT = [sb(f"PT{i}", [128, 512], BF16) for i in range(3)]
    IDENT = sb("IDENT", [128, 128], BF16)
    CM = sb("CM", [128, 128], BF16)
    CM2 = sb("CM2", [128, 128], BF16)
    IOD = sb("IOD", [128, 128], F32)
    GT = sb("GT", [128, 4, NC], F32)
    SMALL = sb("SMALL", [128, 256], F32)
    SUBG = sb("SUBG", [128, 128], F32)
    LAMW = sb("LAMW", [128, 4, 64], F32)
    ESINK = sb("ESINK", [128, 32], F32)
    OW = [sb(f"OW{i}", [128, 128], F32) for i in range(2)]
    OW2 = [sb(f"OW2{i}", [128, 128], F32) for i in range(2)]
    YB = [sb(f"YB{i}", [128, 4, 128], BF16) for i in range(2)]
    SZS = [sb(f"SZS{i}", [128, 512], BF16) for i in range(2)]
    GATE = sb("GATE", [128, 16, 8], F32)
    GATE2 = sb("GATE2", [128, 16, 8], F32)
    MASKV = sb("MASKV", [128, 16, 8], BF16)
    TOP8 = sb("TOP8", [128, 16, 8], F32)
    PASTM = sb("PASTM", [128, 16, 8], F32)
    PAST01 = sb("PAST01", [128, 16, 8], F32)
    OWN01 = sb("OWN01", [128, 16, 8], F32)
    KMEAN = sb("KMEAN", [128, 8], F32)
    KMEANB = sb("KMEANB", [128, 8], BF16)
    OWG = [sb(f"OWG{i}", [128, 512], F32) for i in range(2)]
    bOWG = [Buf(), Buf()]
    S5P = sb("S5P", [128, 8, 64], F32)
    S5D = sb("S5D", [128, 2, NC], F32)
    CAR = sb("CAR", [128, 8], F32)
    MSK = sb("MSK", [128, 8], F32)

    psA = [ps(f"psA{i}", [128, 512], F32) for i in range(2)]
    psG = [ps(f"psG{i}", [128, 512], F32) for i in range(1)]
    psO = ps("psO", [128, 4, 512], F32)
    psT = [ps(f"psT{i}", [128, 8, 128], BF16) for i in range(1)]

    bA = [Buf("psA0"), Buf("psA1")]
    bG = [Buf("psG0")]
    bT = [Buf("psT0")]
    bO = [[Buf(f"psO{j}")] * 2 for j in range(4)]
    bOall = Buf("psOall")

    R2b = R2[:, :].bitcast(BF16)
    VP = R2b[:, 0:16 * 16 * 129].rearrange("p (t h d) -> p t h d", t=16, h=16)
    VPs = R2b[:, 0:16 * 4 * 65].rearrange("p (t h d) -> p t h d", t=16, h=4)
    XT = [R2[:, 0:2048], R2[:, 2048:4096]]
    XN = [R2b[:, 8192:10240], R2b[:, 10240:12288]]
    JUNK = R2b[:, 12288:14336]
    PG = R2[:, 7168:9216]
    TT = R2[:, 9216:11264]
    Yt = [R2b[:, 22528:24576], R2b[:, 24576:26624]]
    SZt = [R2b[:, 26624:28672], R2b[:, 28672:30720]]
    YG = R2b[:, 30720:32768]
    YGT = [R2b[:, 32768:33024].rearrange("p (c t) -> p c t", c=2)]
    bXT = [Buf(), Buf()]
    bXN = [Buf(), Buf()]
    bJ = Buf()
    bPG = Buf()
    bTT = Buf()
    bYt = [Buf(), Buf()]
    bSZt = [Buf(), Buf()]
    bYG = Buf()
    bSS = [Buf(), Buf()]
    YGTv = R2b[:, 8192:10240].rearrange("p (c t) -> p c t", c=16)
    bYGT = Buf()

    bR1 = [Buf(f"R1_{c}") for c in range(NC)]
    bWS = [Buf("WS0"), Buf("WS1")]
    bSTG = [Buf("STG0"), Buf("STG1")]
    bQT = [Buf()] * 2
    bKT = [Buf()] * 2
    bAUXQ = [Buf()] * 2
    bPT = [Buf(), Buf(), Buf()]
    bVP = [Buf(f"VP{t}") for t in range(NT)]
    bOW = [Buf(), Buf()]
    bOW2 = [Buf(), Buf()]
    bYB = [Buf(), Buf()]
    bSZS = [Buf(), Buf()]
    bSM = Buf("small")
    bConst = Buf("const")
    bX = [[Buf(f"x{s}_{i}") for i in range(NT)] for s in range(NSEQ)]
    bSZd = [Buf(f"szd{i}") for i in range(NT)]
    bYd = [Buf(f"yd{i}") for i in range(4)]
    bGate = Buf()
    bMaskv = Buf()

    st = {"ws": 0, "pt": 0, "pa": 0, "ev": 0, "stg": 0}

    C_SS, C_RSTD, C_LAM, C_R0, C_R1, C_SS2, C_TMP = 0, 8, 16, 24, 32, 40, 48

    def evac_engine():
        st["ev"] ^= 1
        return "act" if st["ev"] else "dve"

    def copy_op(eng, out_ap, in_ap, reads, writes, scale=None):
        if eng == "act":
            if scale is None:
                P.op("act", lambda e: e.copy(out=out_ap, in_=in_ap), reads, writes)
            else:
                P.op("act", lambda e: e.mul(out=out_ap, in_=in_ap, mul=float(scale)), reads, writes)
        else:
            if scale is None:
                P.op("dve", lambda e: e.tensor_copy(out=out_ap, in_=in_ap), reads, writes)
            else:
                P.op("dve", lambda e: e.tensor_scalar_mul(out=out_ap, in0=in_ap, scalar1=float(scale)),
                     reads, writes)

    def setup():
        P.op("pool", lambda e: e.iota(IOD[:], pattern=[[1, 128]], base=0, channel_multiplier=-1,
                                      allow_small_or_imprecise_dtypes=True), (), (bConst,))
        P.op("dve", lambda e: e.tensor_single_scalar(out=IDENT[:], in_=IOD[:], scalar=0.0, op=ALU.is_equal),
             (bConst,), (bConst,))
        P.op("dve", lambda e: e.tensor_scalar(out=CM[:], in0=IOD[:], scalar1=0.0, scalar2=-BIG,
                                              op0=ALU.is_lt, op1=ALU.mult), (bConst,), (bConst,))
        P.op("dve", lambda e: e.tensor_scalar(out=CM2[:], in0=IOD[:], scalar1=0.0, scalar2=-BIG,
                                              op0=ALU.is_ge, op1=ALU.mult), (bConst,), (bConst,))
        T_a = R2b[0:1, 0:2048]
        T_b = R2b[0:1, 2048:4096]
        T_1 = R2b[0:1, 4096:6144]
        T_m = R2b[0:1, 6144:8192]
        bt = Buf()
        P.op("pool", lambda e: e.iota(T_a, pattern=[[128, 16], [0, 128]], base=0, channel_multiplier=0,
                                      allow_small_or_imprecise_dtypes=True), (), (bt,))
        P.op("pool", lambda e: e.iota(T_b, pattern=[[0, 16], [1, 128]], base=0, channel_multiplier=0,
                                      allow_small_or_imprecise_dtypes=True), (), (bt,))
        P.op("dve", lambda e: e.memset(T_1, 1.0), (), (bt,))
        P.op("dve", lambda e: e.memset(T_m, -1.0), (), (bt,))
        for r, src in enumerate((T_a, T_b, T_1, T_1)):
            P.dma("sp", lambda e, r=r, src=src: e.dma_start(out=AUXQ[0][8 + r:9 + r, :], in_=src),
                  (bt,), (bConst,))
        for r, src in enumerate((T_m, T_m, T_a, T_b)):
            P.dma("sp", lambda e, r=r, src=src: e.dma_start(out=AUXK[8 + r:9 + r, :], in_=src),
                  (bt,), (bConst,))
        BLK = R2[0:8, 4096:6144]
        BLK2 = R2[0:8, 6144:8192]
        P.op("pool", lambda e: e.iota(BLK, pattern=[[1, 2048]], base=0, channel_multiplier=-256,
                                      allow_small_or_imprecise_dtypes=True), (bt,), (bt,))
        P.op("dve", lambda e: e.tensor_scalar(out=BLK2, in0=BLK, scalar1=0.0, scalar2=None, op0=ALU.is_ge),
             (bt,), (bt,))
        P.op("dve", lambda e: e.tensor_scalar(out=BLK, in0=BLK, scalar1=256.0, scalar2=None, op0=ALU.is_lt),
             (bt,), (bt,))
        P.op("dve", lambda e: e.tensor_tensor(out=AUXK[0:8, :], in0=BLK, in1=BLK2, op=ALU.mult),
             (bt,), (bConst,))
        NB = R2[:, 8192:8320].rearrange("p (t n) -> p t n", t=16)
        TB = R2[:, 8320:8448].rearrange("p (t n) -> p t n", t=16)
        P.op("pool", lambda e: e.iota(NB, pattern=[[0, 16], [1, 8]], base=0, channel_multiplier=0,
                                      allow_small_or_imprecise_dtypes=True), (), (bt,))
        P.op("pool", lambda e: e.iota(TB, pattern=[[1, 16], [0, 8]], base=0, channel_multiplier=0,
                                      allow_small_or_imprecise_dtypes=True), (), (bt,))
        D2 = R2[:, 8448:8576].rearrange("p (t n) -> p t n", t=16)
        P.op("dve", lambda e: e.scalar_tensor_tensor(out=D2, in0=NB, scalar=-2.0, in1=TB,
                                                     op0=ALU.mult, op1=ALU.add), (bt,), (bt,))
        P.op("dve", lambda e: e.tensor_single_scalar(out=PAST01[:], in_=D2, scalar=2.0, op=ALU.is_ge),
             (bt,), (bConst,))
        P.op("dve", lambda e: e.tensor_scalar(out=PASTM[:], in0=PAST01[:], scalar1=1e30, scalar2=-1e30,
                                              op0=ALU.mult, op1=ALU.add), (bConst,), (bConst,))
        TMPO = R2[:, 8576:8704].rearrange("p (t n) -> p t n", t=16)
        P.op("dve", lambda e: e.tensor_single_scalar(out=TMPO, in_=D2, scalar=0.0, op=ALU.is_ge), (bt,), (bt,))
        P.op("dve", lambda e: e.tensor_single_scalar(out=OWN01[:], in_=D2, scalar=1.0, op=ALU.is_le),
             (bt,), (bConst,))
        P.op("dve", lambda e: e.tensor_tensor(out=OWN01[:], in0=OWN01[:], in1=TMPO, op=ALU.mult),
             (bt, bConst), (bConst,))
        P.dma("sp", lambda e: e.dma_start(out=GT[:], in_=pre_gT.rearrange("l p c -> p l c")), (), (bConst,))
        P.fence()

    def phase_a(s, l):
        xsrc = x_in if l == LAYERS[0] else out
        for i in range(NT):
            k = i % 2
            P.dma("sp", lambda e, i=i, k=k: e.dma_start(out=XT[k], in_=xsrc[s, 128 * i:128 * i + 128, :]),
                  (bX[s][i],), (bXT[k],))
            ssc = SMALL[:, C_SS + k:C_SS + k + 1]
            rsc = SMALL[:, C_RSTD + k:C_RSTD + k + 1]
            P.op("act", lambda e, k=k, ssc=ssc: e.activation(out=JUNK, in_=XT[k], func=AF.Square, accum_out=ssc),
                 (bXT[k],), (bJ, bSS[k]))
            P.op("dve", lambda e, ssc=ssc, rsc=rsc: e.tensor_scalar(out=rsc, in0=ssc, scalar1=1.0 / D, scalar2=EPS,
                                                                     op0=ALU.mult, op1=ALU.add), (bSS[k],), (bSS[k],))
            P.op("act", lambda e, rsc=rsc: e.activation(out=rsc, in_=rsc, func=AF.Sqrt), (bSS[k],), (bSS[k],))
            P.op("dve", lambda e, rsc=rsc: e.reciprocal(out=rsc, in_=rsc), (bSS[k],), (bSS[k],))
            P.op("dve", lambda e, k=k, rsc=rsc: e.tensor_scalar(out=XN[k], in0=XT[k], scalar1=rsc, scalar2=None,
                                                                 op0=ALU.mult), (bXT[k], bSS[k]), (bXN[k],))
            for g in range(2):
                for j in range(8):
                    c = 8 * g + j
                    P.op("pe", lambda e, k=k, c=c, j=j: e.transpose(out=psT[0][:, j, :],
                                                                     in_=XN[k][:, 128 * c:128 * c + 128],
                                                                     identity=IDENT[:]),
                         (bXN[k], bConst), (bT[0],))
                gin = GT[:, l, 8 * g:8 * g + 8].unsqueeze(2).to_broadcast([128, 8, 128])
                P.op("dve", lambda e, g=g, i=i, gin=gin: e.tensor_tensor(
                    out=R1[:, 8 * g:8 * g + 8, 128 * i:128 * i + 128], in0=psT[0][:, :, :], in1=gin, op=ALU.mult),
                    (bT[0], bConst), tuple(bR1[8 * g:8 * g + 8]))

    def load_w_to(w_ap, row0, col0, ncols, dst_fn, dbufs):
        k = st["stg"]
        st["stg"] ^= 1
        src = w_ap[row0:row0 + 1024, col0:col0 + ncols].rearrange("(c p) n -> p c n", p=128)
        P.dma("sp", lambda e: e.dma_start(out=STG[k][:, :, 0:ncols], in_=src), (), (bSTG[k],))
        P.op("pool", lambda e: e.tensor_copy(out=dst_fn(), in_=STG[k][:, :, 0:ncols]), (bSTG[k],), dbufs)

    def load_w(w_ap, col0, ncols, dst_col=0, slot=None):
        if slot is None:
            slot = st["ws"]
            st["ws"] ^= 1
        for g in range(2):
            load_w_to(w_ap, 1024 * g, col0, ncols,
                      lambda g=g: WS[slot][:, 8 * g:8 * g + 8, dst_col:dst_col + ncols], (bWS[slot],))
        return slot

    def proj_tok(w_ap, col0, ncols, consume, slot=None):
        if slot is None:
            slot = load_w(w_ap, col0, ncols)
        for i in range(NT):
            a = st["pa"]
            st["pa"] ^= 1
            for c in range(NC):
                P.op("pe", lambda e, a=a, c=c, i=i: e.matmul(psA[a][:, 0:ncols], lhsT=R1[:, c, 128 * i:128 * i + 128],
                                                             rhs=WS[slot][:, c, 0:ncols], start=(c == 0),
                                                             stop=(c == NC - 1)),
                     (bR1[c], bWS[slot]), (bA[a],))
            consume(i, psA[a][:, 0:ncols], bA[a])

    def proj_feat(slot, wcol, M, dst_ap_fn, dbuf, scale=None, prow=0):
        for tb in range(4):
            a = st["pa"]
            st["pa"] ^= 1
            for c in range(NC):
                P.op("pe", lambda e, a=a, c=c, tb=tb: e.matmul(psA[a][prow:prow + M, :],
                                                               lhsT=WS[slot][:, c, wcol:wcol + M],
                                                               rhs=R1[:, c, 512 * tb:512 * tb + 512],
                                                               start=(c == 0), stop=(c == NC - 1)),
                     (bR1[c], bWS[slot]), (bA[a],))
            if isinstance(scale, (list, tuple)):
                eng = evac_engine()
                for hh, sc in enumerate(scale):
                    copy_op(eng, dst_ap_fn(tb)[64 * hh:64 * hh + 64, :], psA[a][64 * hh:64 * hh + 64, :],
                            (bA[a],), (dbuf,), sc)
            else:
                copy_op(evac_engine(), dst_ap_fn(tb), psA[a][prow:prow + M, :], (bA[a],), (dbuf,), scale)

    def proj_z(w_ap, zcol0):
        for j in range(8):
            def consume(i, pap, pbuf, j=j):
                k = (i + j) % 2
                P.op("act", lambda e, k=k, pap=pap: e.activation(out=SZS[k][:, 0:256], in_=pap, func=AF.Silu),
                     (pbuf,), (bSZS[k],))
                P.dma("sp", lambda e, k=k, i=i, j=j: e.dma_start(out=SZ[128 * i:128 * i + 128, 256 * j:256 * j + 256],
                                                                 in_=SZS[k][:, 0:256]), (bSZS[k],), (bSZd[i],))
            if j == 0:
                zslots = {0: load_w(w_ap, zcol0, 256)}
            if j + 1 < 8:
                zslots[j + 1] = load_w(w_ap, zcol0 + 256 * (j + 1), 256)
            proj_tok(w_ap, zcol0 + 256 * j, 256, consume, slot=zslots[j])

    def attention_head(qsrc, ksrc, krow, nk, vrhs_fn, dv, items_fn, slope, auxq, auxk, naux, finalize,
                       nmaps=1, qb_list=range(4), bq=None, bk=None, baux=None):
        for qb in qb_list:
            items = items_fn(qb)
            sinfo = {}

            def emit_s(n):
                m, kt, jq0, masks, jq1 = items[n]
                a = st["pa"]
                st["pa"] ^= 1
                sinfo[n] = a
                q0 = 512 * qb
                r0 = krow(m)
                pieces = []
                col = 128 * jq0
                for mk in masks:
                    pieces.append((col, col + 128, mk))
                    col += 128
                if col < 128 * jq1:
                    pieces.append((col, 128 * jq1, None))
                for (lo, hi, mk) in pieces:
                    P.op("pe", lambda e, a=a, m=m, kt=kt, lo=lo, hi=hi, r0=r0: e.matmul(
                        psA[a][:, lo:hi], lhsT=ksrc(m)[r0:r0 + nk, 128 * kt:128 * kt + 128],
                        rhs=qsrc(m)[r0:r0 + nk, q0 + lo:q0 + hi], start=True, stop=False),
                        (bq, bk), (bA[a],))
                    P.op("pe", lambda e, a=a, kt=kt, lo=lo, hi=hi, mk=mk: e.matmul(
                        psA[a][:, lo:hi], lhsT=auxk[0:naux, 128 * kt:128 * kt + 128],
                        rhs=auxq[0:naux, q0 + lo:q0 + hi], start=False, stop=(mk is None)),
                        (baux, bConst), (bA[a],))
                    if mk is not None:
                        P.op("pe", lambda e, a=a, lo=lo, hi=hi, mk=mk: e.matmul(
                            psA[a][:, lo:hi], lhsT=IDENT[:], rhs=mk[:], start=False, stop=True),
                            (bConst,), (bA[a],))

            def emit_rest(n):
                m, kt, jq0, masks, jq1 = items[n]
                a = sinfo[n]
                p = st["pt"]
                st["pt"] = (st["pt"] + 1) % 3
                lo = 128 * jq0
                hi = 128 * jq1
                sl = float(slope(m))
                P.op("act", lambda e, a=a, p=p, lo=lo, hi=hi, sl=sl: e.activation(
                    out=PT[p][:, lo:hi], in_=psA[a][:, lo:hi], func=AF.Exp, scale=sl),
                     (bA[a],), (bPT[p],))
                for jq in range(jq0, jq1):
                    tq = 4 * qb + jq
                    first = first_kt(m, tq) == kt
                    last = (kt == tq)
                    P.op("pe", lambda e, p=p, jq=jq, m=m, kt=kt, first=first, last=last: e.matmul(
                        psO[:, jq, 256 * m:256 * m + dv + 1], lhsT=PT[p][:, 128 * jq:128 * jq + 128],
                        rhs=vrhs_fn(kt), start=first, stop=last),
                        (bPT[p], bVP[kt]), (bO[jq][m],))

            first_kt = items_fn.first_kt
            if items:
                emit_s(0)
            for n in range(len(items)):
                if n + 1 < len(items):
                    emit_s(n + 1)
                emit_rest(n)
            for jq in range(4):
                finalize(qb, jq)

    def norm_tail(qb, jq, h, dv, obuf_ap, ob):
        pass

    def y_store(qb, h, width, k):
        P.dma("sp", lambda e: e.dma_start(
            out=YS[512 * qb:512 * qb + 512, width * h:width * h + width].rearrange("(j p) d -> p j d", p=128),
            in_=YB[k][:, :, 0:width]), (bYB[k],), (bYd[qb],))

    def layer_diff(s, l):
        w_in = W["diff_w_in"]
        lam_init = 0.8 - 0.6 * math.exp(-0.3 * l)
        slopes = alibi_slopes(16)
        scale = 64 ** -0.5
        P.dma("sp", lambda e: e.dma_start(out=LAMW[:].rearrange("p a b -> p (a b)"),
                                          in_=diff_l.rearrange("a b -> (a b)").partition_broadcast(128)),
              (), (bSM,))
        lw = SMALL[:, C_TMP:C_TMP + 2]
        P.op("dve", lambda e: e.tensor_tensor(out=LAMW[:, 0, :], in0=LAMW[:, 0, :], in1=LAMW[:, 1, :], op=ALU.mult),
             (bSM,), (bSM,))
        P.op("dve", lambda e: e.tensor_tensor(out=LAMW[:, 2, :], in0=LAMW[:, 2, :], in1=LAMW[:, 3, :], op=ALU.mult),
             (bSM,), (bSM,))
        P.op("dve", lambda e: e.reduce_sum(out=lw[:, 0:1], in_=LAMW[:, 0, :], axis=AX.X), (bSM,), (bSM,))
        P.op("dve", lambda e: e.reduce_sum(out=lw[:, 1:2], in_=LAMW[:, 2, :], axis=AX.X), (bSM,), (bSM,))
        P.op("act", lambda e: e.activation(out=lw, in_=lw, func=AF.Exp), (bSM,), (bSM,))
        lam = SMALL[:, C_LAM:C_LAM + 1]
        P.op("dve", lambda e: e.scalar_tensor_tensor(out=lam, in0=lw[:, 0:1], scalar=lam_init, in1=lw[:, 1:2],
                                                     op0=ALU.add, op1=ALU.subtract), (bSM,), (bSM,))
        P.dma("sp", lambda e: e.dma_start(out=SUBG[:], in_=diff_subln[0, :].partition_broadcast(128)), (), (bSM,))
        P.op("dve", lambda e: e.tensor_scalar_mul(out=SUBG[:], in0=SUBG[:], scalar1=1.0 - lam_init), (bSM,), (bSM,))

        P.op("dve", lambda e: e.memset(AUXQ[0][0:8, :], 0.0), (), (bAUXQ[0],))
        phase_a(s, l)
        P.fence()
        P.op("dve", lambda e: e.memset(R2b[:, 0:16 * 16 * 129], 1.0), (), tuple(bVP))
        for j in range(8):
            def consume(i, pap, pbuf, j=j):
                eng = evac_engine()
                copy_op(eng, VP[:, i, 2 * j:2 * j + 2, 0:128], pap.rearrange("p (h d) -> p h d", h=2),
                        (pbuf,), (bVP[i],))
            proj_tok(w_in, 4096 + 256 * j, 256, consume)
        proj_z(w_in, 6144)

        def items_fn(qb):
            its = []
            for m in range(2):
                for kt in range(4 * qb + 4):
                    if kt < 4 * qb:
                        its.append((m, kt, 0, [], 4))
                    else:
                        its.append((m, kt, kt - 4 * qb, [CM], 4))
            return its
        items_fn.first_kt = lambda m, tq: 0

        for h in range(16):
            slot = st["ws"]
            st["ws"] ^= 1
            load_w(w_in, 128 * h, 128, 0, slot)
            load_w(w_in, 2048 + 128 * h, 128, 128, slot)
            k = h % 2
            proj_feat(slot, 0, 128, lambda tb, k=k: QT[k][:, 512 * tb:512 * tb + 512], bQT[k],
                      scale=scale / slopes[h])
            proj_feat(slot, 128, 128, lambda tb, k=k: KT[k][:, 512 * tb:512 * tb + 512], bKT[k])

            def finalize(qb, jq, h=h):
                o = jq % 2
                r0 = SMALL[:, C_R0 + o:C_R0 + o + 1]
                r1 = SMALL[:, C_R1 + o:C_R1 + o + 1]
                ss = SMALL[:, C_SS2 + o:C_SS2 + o + 1]
                P.op("dve", lambda e: e.reciprocal(out=r0, in_=psO[:, jq, 128:129]), (bO[jq][0],), (bOW[o],))
                P.op("dve", lambda e: e.reciprocal(out=r1, in_=psO[:, jq, 256 + 128:256 + 129]), (bO[jq][1],),
                     (bOW[o],))
                P.op("dve", lambda e: e.tensor_tensor(out=r1, in0=r1, in1=lam, op=ALU.mult), (bOW[o], bSM), (bOW[o],))
                P.op("dve", lambda e: e.tensor_scalar(out=OW2[o][:], in0=psO[:, jq, 256:256 + 128], scalar1=r1,
                                                      scalar2=None, op0=ALU.mult), (bO[jq][1], bOW[o]), (bOW2[o],))
                P.op("dve", lambda e: e.scalar_tensor_tensor(out=OW[o][:], in0=psO[:, jq, 0:128], scalar=r0,
                                                             in1=OW2[o][:], op0=ALU.mult, op1=ALU.subtract),
                     (bO[jq][0], bOW2[o], bOW[o]), (bOW[o],))
                P.op("act", lambda e: e.activation(out=OW2[o][:], in_=OW[o][:], func=AF.Square, accum_out=ss),
                     (bOW[o],), (bOW2[o],))
                P.op("dve", lambda e: e.tensor_scalar(out=ss, in0=ss, scalar1=1.0 / 128, scalar2=EPS,
                                                      op0=ALU.mult, op1=ALU.add), (bOW2[o],), (bOW2[o],))
                P.op("act", lambda e: e.activation(out=ss, in_=ss, func=AF.Sqrt), (bOW2[o],), (bOW2[o],))
                P.op("dve", lambda e: e.reciprocal(out=ss, in_=ss), (bOW2[o],), (bOW2[o],))
                kb = (h * 4 + qb) % 2
                P.op("dve", lambda e: e.scalar_tensor_tensor(out=YB[kb][:, jq, :], in0=OW[o][:], scalar=ss,
                                                             in1=SUBG[:], op0=ALU.mult, op1=ALU.mult),
                     (bOW[o], bOW2[o], bSM), (bYB[kb],))
                if jq == 3:
                    y_store(qb, h, 128, kb)

            attention_head(lambda m, k=k: QT[k], lambda m, k=k: KT[k], lambda m: 64 * m, 64,
                           lambda kt, h=h: VP[:, kt, h, :], 128, items_fn, (lambda m, h=h: slopes[h]), AUXQ[0], AUXK, 12, finalize,
                           nmaps=2, bq=bQT[k], bk=bKT[k], baux=bAUXQ[0])
        P.fence()
        phase_d(s, l, W["diff_w_out"])

    def layer_s5(s, l):
        w_in = W["s5_w_in"]
        TWO_PI = 6.283185
        INV2PI = 1.0 / (2.0 * math.pi)
        UT = R2b[:, 0:NC * 2048].rearrange("p (c t) -> p c t", c=NC)
        bUT = [Buf(f"UT{c}") for c in range(NC)]
        slots = []
        for tl in (WS[0], WS[1]):
            v = tl[:, :, :].rearrange("p c n -> p (c n)").bitcast(F32)
            slots += [v[:, 512 * k:512 * k + 512] for k in range(4)]
        for tl in (STG[0], STG[1]):
            v = tl[:, :, :].rearrange("p c n -> p (c n)")
            slots += [v[:, 512 * k:512 * k + 512] for k in range(4)]
        bS = [Buf(f"slot{i}") for i in range(16)]
        IOTA_S, COST, SINT, A_sb, B_sb, T13, T2s, T4s, W_re, W_im, Z_re, Z_im = slots[0:12]
        PB = [slots[12].bitcast(BF16)[:, 0:512], slots[12].bitcast(BF16)[:, 512:1024],
              slots[13].bitcast(BF16)[:, 0:512], slots[13].bitcast(BF16)[:, 512:1024]]
        bPB = [Buf(), Buf(), Buf(), Buf()]
        YSf = [slots[14], slots[14]]
        NSINT = slots[15]
        TIi = slots[5].bitcast(I32)

        phase_a(s, l)
        P.fence()
        if DEBUG_STOP[0] == 5:
            phase_d(s, l, W["s5_w_out"], s5=True)
            return
        for c in range(NC):
            slot = st["ws"]
            st["ws"] ^= 1
            load_w(w_in, 128 * c, 128, 0, slot)
            load_w(w_in, 2048 + 128 * c, 128, 128, slot)
            proj_feat(slot, 0, 128, lambda tb, c=c: UT[:, c, 512 * tb:512 * tb + 512], bUT[c])
            for tb in range(4):
                a = st["pa"]
                st["pa"] ^= 1
                for ci in range(NC):
                    P.op("pe", lambda e, a=a, ci=ci, tb=tb, slot=slot: e.matmul(
                        psA[a][:, :], lhsT=WS[slot][:, ci, 128:256], rhs=R1[:, ci, 512 * tb:512 * tb + 512],
                        start=(ci == 0), stop=(ci == NC - 1)), (bR1[ci], bWS[slot]), (bA[a],))
                k = tb % 2
                P.op("act", lambda e, a=a, k=k: e.activation(out=SZS[k][:], in_=psA[a][:, :], func=AF.Silu),
                     (bA[a],), (bSZS[k],))
                P.dma("sp", lambda e, k=k, c=c, tb=tb: e.dma_start(out=ZT[c, :, 512 * tb:512 * tb + 512], in_=SZS[k][:]),
                      (bSZS[k],), (bSZd[c],))
        P.fence()
        if DEBUG_STOP[0] == 1:
            return
        R1f = R1[:, :, :].rearrange("p c t -> p (c t)")
        TB = [[R1f[:, 2048 * (2 * ri + v):2048 * (2 * ri + v) + 2048].rearrange("p (c n) -> p c n", c=NC)
               for v in range(2)] for ri in range(2)]
        CTre = R1f[:, 8192:16384].rearrange("p (g n) -> p g n", g=64)
        CTim = R1f[:, 16384:24576].rearrange("p (g n) -> p g n", g=64)
        scr = R1f[:, 24576:32768].bitcast(F32)
        Xs = [scr[:, 1024 * k:1024 * k + 1024] for k in range(4)]
        bt = Buf("s5tab")
        big = [slots[2 * k] for k in range(8)]
        P.dma("sp", lambda e: e.dma_start(out=X3[0], in_=s5p["s5_are_X"]), (), (bt,))
        P.dma("sp", lambda e: e.dma_start(out=X3[1], in_=s5p["s5_aim_X"]), (), (bt,))
        P.dma("sp", lambda e: e.dma_start(out=S5D[:, 0, :], in_=s5p["s5_dT"]), (), (bt,))
        P.dma("sp", lambda e: e.dma_start(out=S5D[:, 1, :], in_=s5p["s5_b_gluT"]), (), (bt,))
        LDT = SMALL[:, 64:80]
        P.dma("sp", lambda e: e.dma_start(out=LDT, in_=s5p["s5_ldt_X"]), (), (bt,))
        P.op("act", lambda e: e.activation(out=LDT, in_=LDT, func=AF.Exp), (bt,), (bt,))
        dtb = LDT.unsqueeze(2).to_broadcast([128, NC, 64])
        def pair(i):
            tl = (WS[0], WS[1], STG[0], STG[1])[i // 2]
            v = tl[:, :, :].rearrange("p c n -> p (c n)")
            if i < 4:
                v = v.bitcast(F32)
            return v[:, 1024 * (i % 2):1024 * (i % 2) + 1024]
        X = [pair(i) for i in range(4, 8)]
        X3 = [x.rearrange("p (c n) -> p c n", c=NC) for x in X]
        Y = Xs + [pair(i) for i in range(4)]
        Y3 = [y.rearrange("p (c n) -> p c n", c=NC) for y in Y]
        TT_ = lambda eng, o, a, b, op: P.op(eng, lambda e: e.tensor_tensor(out=o, in0=a, in1=b, op=op), (bt,), (bt,))
        TS_ = lambda eng, o, a, s1, s2, o0, o1=None: P.op(eng, lambda e: (
            e.tensor_scalar(out=o, in0=a, scalar1=s1, scalar2=s2, op0=o0, op1=o1) if o1 is not None else
            e.tensor_scalar(out=o, in0=a, scalar1=s1, scalar2=None, op0=o0)), (bt,), (bt,))

        def sincos_turns(tp, sin_o, cos_o, tmp_f, tmp_i):
            P.op("dve", lambda e: e.tensor_copy(out=tmp_i, in_=tp), (bt,), (bt,))
            TT_("dve", tmp_f, tp, tmp_i, ALU.subtract)
            P.op("act", lambda e: e.activation(out=sin_o, in_=tmp_f, func=AF.Sin, scale=TWO_PI), (bt,), (bt,))
            TS_("dve", tmp_f, tp, 0.25, None, ALU.add)
            P.op("dve", lambda e: e.tensor_copy(out=tmp_i, in_=tmp_f), (bt,), (bt,))
            TT_("dve", tmp_f, tmp_f, tmp_i, ALU.subtract)
            P.op("act", lambda e: e.activation(out=cos_o, in_=tmp_f, func=AF.Sin, scale=TWO_PI), (bt,), (bt,))

        LR, LI = X[0], X[1]
        TT_("dve", Y3[0], X3[0], dtb, ALU.mult)
        P.op("act", lambda e: e.activation(out=Y[0], in_=Y[0], func=AF.Exp), (bt,), (bt,))
        TT_("dve", Y3[1], X3[1], dtb, ALU.mult)
        TS_("dve", Y[1], Y[1], INV2PI, None, ALU.mult)
        sincos_turns(Y[1], Y[2], Y[3], Y[4], Y[5].bitcast(I32))
        TT_("dve", Y[3], Y[3], Y[0], ALU.mult)
        TT_("dve", Y[2], Y[2], Y[0], ALU.mult)
        TS_("dve", Y[3], Y[3], -1.0, None, ALU.add)
        TT_("dve", Y[0], LR, LR, ALU.mult)
        TT_("dve", Y[1], LI, LI, ALU.mult)
        TT_("dve", Y[0], Y[0], Y[1], ALU.add)
        P.op("dve", lambda e: e.reciprocal(out=Y[0], in_=Y[0]), (bt,), (bt,))
        TT_("dve", Y[4], Y[3], LR, ALU.mult)
        TT_("dve", Y[5], Y[2], LI, ALU.mult)
        TT_("dve", Y[4], Y[4], Y[5], ALU.add)
        TT_("dve", Y[4], Y[4], Y[0], ALU.mult)
        TT_("dve", Y[5], Y[2], LR, ALU.mult)
        TT_("dve", Y[1], Y[3], LI, ALU.mult)
        TT_("dve", Y[5], Y[5], Y[1], ALU.subtract)
        TT_("dve", Y[5], Y[5], Y[0], ALU.mult)
        P.dma("sp", lambda e: e.dma_start(out=X3[2], in_=s5p["s5_bre_X"]), (), (bt,))
        P.dma("sp", lambda e: e.dma_start(out=X3[3], in_=s5p["s5_bim_X"]), (), (bt,))
        TT_("dve", Y[0], Y[4], X[2], ALU.mult)
        TT_("dve", Y[1], Y[5], X[3], ALU.mult)
        TT_("dve", Y[0], Y[0], Y[1], ALU.subtract)
        TT_("dve", Y[2], Y[4], X[3], ALU.mult)
        TT_("dve", Y[3], Y[5], X[2], ALU.mult)
        TT_("dve", Y[2], Y[2], Y[3], ALU.add)
        PI_ = SMALL[:, 80:81].bitcast(I32)
        PJ_ = SMALL[:, 81:82].bitcast(I32)
        PK_ = SMALL[:, 82:83].bitcast(I32)
        P.op("pool", lambda e: e.iota(PI_, pattern=[[0, 1]], base=0, channel_multiplier=1), (bt,), (bt,))
        P.op("dve", lambda e: e.tensor_scalar(out=PJ_, in0=PI_, scalar1=5, scalar2=1, op0=ALU.logical_shift_right,
                                              op1=ALU.bitwise_and), (bt,), (bt,))
        P.op("dve", lambda e: e.tensor_scalar(out=PK_, in0=PI_, scalar1=4, scalar2=1, op0=ALU.logical_shift_right,
                                              op1=ALU.bitwise_and), (bt,), (bt,))
        FJ = SMALL[:, 84:85]
        FK = SMALL[:, 85:86]
        P.op("dve", lambda e: e.tensor_copy(out=FJ, in_=PJ_), (bt,), (bt,))
        P.op("dve", lambda e: e.tensor_copy(out=FK, in_=PK_), (bt,), (bt,))
        for v in range(2):
            for g2 in range(2):
                mc = MSK[:, 2 * v + g2:2 * v + g2 + 1]
                P.op("dve", lambda e, mc=mc, v=v: e.tensor_single_scalar(out=mc, in_=FJ, scalar=float(v), op=ALU.is_equal),
                     (bt,), (bt,))
                P.op("dve", lambda e, mc=mc, g2=g2: e.scalar_tensor_tensor(out=mc, in0=FK, scalar=float(g2), in1=mc,
                                                                          op0=ALU.is_equal, op1=ALU.mult), (bt,), (bt,))
                for ri, src in ((0, Y3[0]), (1, Y3[2])):
                    P.op("dve", lambda e, ri=ri, v=v, g2=g2, mc=mc, src=src: e.tensor_scalar(
                        out=TB[ri][v][:, :, 64 * g2:64 * g2 + 64], in0=src, scalar1=mc, scalar2=None, op0=ALU.mult),
                        (bt,), (bt,))
        THP, RHO, CR, SR, SRN, TMPa, TMPb, TMPc = [S5P[:, k, :] for k in range(8)]
        P.dma("sp", lambda e: e.dma_start(out=TMPa, in_=s5p["s5_ldt_Y"]), (), (bt,))
        P.dma("sp", lambda e: e.dma_start(out=RHO, in_=s5p["s5_are_Y"]), (), (bt,))
        P.dma("sp", lambda e: e.dma_start(out=THP, in_=s5p["s5_aim_Y"]), (), (bt,))
        P.op("act", lambda e: e.activation(out=TMPa, in_=TMPa, func=AF.Exp), (bt,), (bt,))
        TT_("dve", RHO, RHO, TMPa, ALU.mult)
        P.op("act", lambda e: e.activation(out=RHO, in_=RHO, func=AF.Exp), (bt,), (bt,))
        TT_("dve", THP, THP, TMPa, ALU.mult)
        TS_("dve", THP, THP, INV2PI, None, ALU.mult)
        TS_("dve", TMPa, THP, 512.0, None, ALU.mult)
        sincos_turns(TMPa, SR, CR, TMPb, TMPc.bitcast(I32))
        TS_("dve", SRN, SR, -1.0, None, ALU.mult)
        P.op("dve", lambda e: e.memset(R1f[:, 8192:24576], 0.0), (bt,), (bt,))
        CY = [X[2][:, :].rearrange("p (g c) -> p g c", c=16), X[3][:, :].rearrange("p (g c) -> p g c", c=16)]
        P.dma("sp", lambda e: e.dma_start(out=CY[0], in_=s5p["s5_cre_Y"]), (), (bt,))
        P.dma("sp", lambda e: e.dma_start(out=CY[1], in_=s5p["s5_cim_Y"]), (), (bt,))
        for ri, (CT_, sgn) in enumerate(((CTre, 1.0), (CTim, -1.0))):
            CT4 = CT_.rearrange("p (q j) n -> p q j n", j=4)
            CY4 = CY[ri].rearrange("p (q j) c -> p q j c", j=4)
            for j in range(4):
                for g2 in range(2):
                    P.op("dve", lambda e, CT4=CT4, CY4=CY4, j=j, g2=g2, sgn=sgn: e.tensor_scalar_mul(
                        out=CT4[64 * g2:64 * g2 + 64, :, j, 32 * j + 16 * g2:32 * j + 16 * g2 + 16],
                        in0=CY4[64 * g2:64 * g2 + 64, :, j, :], scalar1=sgn), (bt,), (bt,))
        P.fence()
        if DEBUG_STOP[0] == 2:
            return
        P.op("pool", lambda e: e.iota(IOTA_S, pattern=[[1, 512]], base=0, channel_multiplier=0,
                                      allow_small_or_imprecise_dtypes=True), (), (bS[0],))
        for cq in range(DEBUG_STOP[1]):
            for j in range(4):
                gp = 4 * cq + j
                hb = 64 * (j // 2)
                v = j % 2
                thc = THP[:, gp:gp + 1]
                P.op("dve", lambda e, thc=thc: e.tensor_scalar(out=A_sb, in0=IOTA_S, scalar1=thc, scalar2=None,
                                                               op0=ALU.mult), (bS[0], bt), (bS[3],))
                P.op("dve", lambda e: e.tensor_copy(out=TIi, in_=A_sb), (bS[3],), (bS[5],))
                P.op("dve", lambda e: e.tensor_tensor(out=B_sb, in0=A_sb, in1=TIi, op=ALU.subtract),
                     (bS[3], bS[5]), (bS[4],))
                P.op("act", lambda e: e.activation(out=SINT, in_=B_sb, func=AF.Sin, scale=TWO_PI), (bS[4],), (bS[2],))
                P.op("act", lambda e: e.activation(out=NSINT, in_=B_sb, func=AF.Sin, scale=-TWO_PI), (bS[4],), (bS[15],))
                P.op("dve", lambda e: e.tensor_scalar(out=A_sb, in0=A_sb, scalar1=0.25, scalar2=None, op0=ALU.add),
                     (bS[3],), (bS[3],))
                P.op("dve", lambda e: e.tensor_copy(out=TIi, in_=A_sb), (bS[3],), (bS[5],))
                P.op("dve", lambda e: e.tensor_tensor(out=B_sb, in0=A_sb, in1=TIi, op=ALU.subtract),
                     (bS[3], bS[5]), (bS[4],))
                P.op("act", lambda e: e.activation(out=COST, in_=B_sb, func=AF.Sin, scale=TWO_PI), (bS[4],), (bS[1],))
                rho_b = RHO[:, gp:gp + 1].to_broadcast([128, 512])
                for tb in range(0 if DEBUG_STOP[2] == 2 else 4):
                    blk = slice(512 * tb, 512 * tb + 512)
                    P.op("pe", lambda e, cq=cq, hb=hb, v=v, blk=blk: e.matmul(
                        psA[0][:, :], lhsT=TB[0][v][hb:hb + 64, cq, :], rhs=UT[hb:hb + 64, cq, blk],
                        start=True, stop=True), (bt, bUT[cq]), (bA[0],))
                    P.op("pe", lambda e, cq=cq, hb=hb, v=v, blk=blk: e.matmul(
                        psA[1][:, :], lhsT=TB[1][v][hb:hb + 64, cq, :], rhs=UT[hb:hb + 64, cq, blk],
                        start=True, stop=True), (bt, bUT[cq]), (bA[1],))
                    P.op("act", lambda e: e.copy(out=A_sb, in_=psA[0][:, :]), (bA[0],), (bS[3],))
                    P.op("act", lambda e: e.copy(out=B_sb, in_=psA[1][:, :]), (bA[1],), (bS[4],))
                    if DEBUG_STOP[2] == 5:
                        continue
                    P.op("pool", lambda e: e.tensor_tensor(out=T2s, in0=SINT, in1=B_sb, op=ALU.mult),
                         (bS[2], bS[4]), (bS[6],))
                    P.op("pool", lambda e: e.tensor_tensor(out=T4s, in0=SINT, in1=A_sb, op=ALU.mult),
                         (bS[2], bS[3]), (bS[7],))
                    if DEBUG_STOP[2] == 6:
                        continue
                    P.op("dve", lambda e: e.tensor_tensor(out=T13, in0=COST, in1=A_sb, op=ALU.mult),
                         (bS[1], bS[3]), (bS[5],))
                    P.op("dve", lambda e: e.tensor_tensor(out=W_re, in0=T13, in1=T2s, op=ALU.add),
                         (bS[5], bS[6]), (bS[8],))
                    P.op("dve", lambda e: e.tensor_tensor(out=T13, in0=COST, in1=B_sb, op=ALU.mult),
                         (bS[1], bS[4]), (bS[5],))
                    P.op("dve", lambda e: e.tensor_tensor(out=W_im, in0=T13, in1=T4s, op=ALU.subtract),
                         (bS[5], bS[7]), (bS[9],))
                    if DEBUG_STOP[2] == 3:
                        continue
                    ini_re = 0.0 if tb == 0 else CAR[:, 0:1]
                    ini_im = 0.0 if tb == 0 else CAR[:, 1:2]
                    P.op("dve", lambda e, ini_re=ini_re, rho_b=rho_b: e.tensor_tensor_scan(
                        out=Z_re, data0=rho_b, data1=W_re, initial=ini_re, op0=ALU.mult, op1=ALU.add),
                        (bS[8], bt, bSM), (bS[10],))
                    P.op("dve", lambda e, ini_im=ini_im, rho_b=rho_b: e.tensor_tensor_scan(
                        out=Z_im, data0=rho_b, data1=W_im, initial=ini_im, op0=ALU.mult, op1=ALU.add),
                        (bS[9], bt, bSM), (bS[11],))
                    if DEBUG_STOP[2] == 4:
                        continue
                    P.op("dve", lambda e: e.tensor_tensor(out=PB[0], in0=COST, in1=Z_re, op=ALU.mult),
                         (bS[1], bS[10]), (bPB[0],))
                    P.op("dve", lambda e: e.tensor_tensor(out=PB[2], in0=COST, in1=Z_im, op=ALU.mult),
                         (bS[1], bS[11]), (bPB[2],))
                    P.op("pool", lambda e: e.tensor_tensor(out=PB[1], in0=NSINT, in1=Z_im, op=ALU.mult),
                         (bS[15], bS[11]), (bPB[1],))
                    P.op("pool", lambda e: e.tensor_tensor(out=PB[3], in0=SINT, in1=Z_re, op=ALU.mult),
                         (bS[2], bS[10]), (bPB[3],))
                    for q, (CT_, pb) in enumerate(((CTre, 0), (CTre, 1), (CTim, 2), (CTim, 3))):
                        P.op("pe", lambda e, CT_=CT_, pb=pb, gp=gp, tb=tb, j=j, q=q: e.matmul(
                            psO[:, tb, :], lhsT=CT_[:, gp, :], rhs=PB[pb], start=(j == 0 and q == 0),
                            stop=(j == 3 and q == 3)), (bt, bPB[pb]), (bO[tb][0],))
                    if tb < 3:
                        crc, src, srnc = CR[:, gp:gp + 1], SR[:, gp:gp + 1], SRN[:, gp:gp + 1]
                        zr, zi = Z_re[:, 511:512], Z_im[:, 511:512]
                        P.op("dve", lambda e, zr=zr, crc=crc: e.tensor_scalar(out=CAR[:, 2:3], in0=zr, scalar1=crc,
                                                                              scalar2=None, op0=ALU.mult),
                             (bS[10], bt), (bSM,))
                        P.op("dve", lambda e, zr=zr, src=src: e.tensor_scalar(out=CAR[:, 3:4], in0=zr, scalar1=src,
                                                                              scalar2=None, op0=ALU.mult),
                             (bS[10], bt), (bSM,))
                        P.op("dve", lambda e, zi=zi, srnc=srnc: e.scalar_tensor_tensor(
                            out=CAR[:, 0:1], in0=zi, scalar=srnc, in1=CAR[:, 2:3], op0=ALU.mult, op1=ALU.add),
                            (bS[11], bt, bSM), (bSM,))
                        P.op("dve", lambda e, zi=zi, crc=crc: e.scalar_tensor_tensor(
                            out=CAR[:, 1:2], in0=zi, scalar=crc, in1=CAR[:, 3:4], op0=ALU.mult, op1=ALU.add),
                            (bS[11], bt, bSM), (bSM,))
            for tb in range(0 if DEBUG_STOP[2] in (2, 3, 4, 5, 6) else 4):
                k = tb % 2
                blk = slice(512 * tb, 512 * tb + 512)
                P.op("dve", lambda e, cq=cq, tb=tb, k=k, blk=blk: e.scalar_tensor_tensor(
                    out=YSf[k], in0=UT[:, cq, blk], scalar=S5D[:, 0, cq:cq + 1], in1=psO[:, tb, :],
                    op0=ALU.mult, op1=ALU.add), (bUT[cq], bO[tb][0], bt), (bS[14],))
                P.op("act", lambda e, cq=cq, k=k, blk=blk: e.activation(out=UT[:, cq, blk], in_=YSf[k],
                                                                        func=(AF.Copy if DEBUG_STOP[2] == 1 else AF.Gelu_apprx_tanh)),
                     (bS[14],), (bUT[cq],))
        P.fence()
        if DEBUG_STOP[0] == 3:
            return
        for co in range(NC):
            slot = st["ws"]
            st["ws"] ^= 1
            load_w(W["s5_w_glu"], 128 * co, 128, 0, slot)
            for tb in range(4):
                blk = slice(512 * tb, 512 * tb + 512)
                a = st["pa"]
                st["pa"] ^= 1
                k = tb % 2
                P.dma("sp", lambda e, k=k, co=co, blk=blk: e.dma_start(out=SZS[k][:], in_=ZT[co, :, blk]),
                      (bSZd[co],), (bSZS[k],))
                for ci in range(NC):
                    P.op("pe", lambda e, a=a, ci=ci, blk=blk, slot=slot: e.matmul(
                        psA[a][:, :], lhsT=WS[slot][:, ci, 0:128], rhs=UT[:, ci, blk],
                        start=(ci == 0), stop=(ci == NC - 1)), (bUT[ci], bWS[slot]), (bA[a],))
                o = tb % 2
                P.op("act", lambda e, a=a, o=o, co=co: e.activation(out=OWG[o][:], in_=psA[a][:, :], func=AF.Sigmoid,
                                                                    bias=S5D[:, 1, co:co + 1]),
                     (bA[a], bt), (bOWG[o],))
                P.op("dve", lambda e, o=o, co=co, blk=blk: e.tensor_tensor(out=OWG[o][:], in0=OWG[o][:], in1=UT[:, co, blk],
                                                                           op=ALU.mult), (bOWG[o], bUT[co]), (bOWG[o],))
                P.op("dve", lambda e, o=o, co=co, blk=blk, k=k: e.tensor_tensor(out=R1[:, co, blk], in0=OWG[o][:],
                                                                                in1=SZS[k][:], op=ALU.mult),
                     (bOWG[o], bSZS[k]), (bR1[co],))
        P.fence()
        if DEBUG_STOP[0] == 4:
            return
        phase_d(s, l, W["s5_w_out"], s5=True)


    def layer_swa(s, l):
        w_in = W["swa_w_in"]
        slopes = alibi_slopes(32)
        scale = 64 ** -0.5
        P.dma("sp", lambda e: e.dma_start(out=ESINK[:], in_=swa_sinks[0, :].partition_broadcast(128)), (), (bSM,))
        P.op("act", lambda e: e.activation(out=ESINK[:], in_=ESINK[:], func=AF.Exp), (bSM,), (bSM,))
        P.op("dve", lambda e: e.memset(AUXQ[0][0:8, :], 0.0), (), (bAUXQ[0],))
        phase_a(s, l)
        P.fence()
        P.op("dve", lambda e: e.memset(R2b[:, 0:16 * 4 * 65], 1.0), (), tuple(bVP))

        def consume_v(i, pap, pbuf):
            copy_op(evac_engine(), VPs[:, i, :, 0:64], pap.rearrange("p (h d) -> p h d", h=4), (pbuf,), (bVP[i],))
        proj_tok(w_in, 2304, 256, consume_v)
        proj_z(w_in, 2560)

        def items_fn(qb):
            its = []
            for m in range(2):
                if qb > 0:
                    its.append((m, 4 * qb - 1, 0, [CM2], 1))
                for j in range(4):
                    its.append((m, 4 * qb + j, j, [CM, CM2] if j < 3 else [CM], min(j + 2, 4)))
            return its
        items_fn.first_kt = lambda m, tq: max(tq - 1, 0)

        for kv in range(4):
            slot = st["ws"]
            st["ws"] ^= 1
            load_w(w_in, 2048 + 64 * kv, 64, 0, slot)
            proj_feat(slot, 0, 64, lambda tb: KT[0][0:64, 512 * tb:512 * tb + 512], bKT[0], prow=0)
            proj_feat(slot, 0, 64, lambda tb: KT[0][64:128, 512 * tb:512 * tb + 512], bKT[0], prow=64)
            for c in range(4 * kv, 4 * kv + 4):
                slot = st["ws"]
                st["ws"] ^= 1
                load_w(w_in, 128 * c, 128, 0, slot)
                proj_feat(slot, 0, 128, lambda tb: QT[0][:, 512 * tb:512 * tb + 512], bQT[0],
                          scale=[scale / slopes[2 * c], scale / slopes[2 * c + 1]])

                def finalize(qb, jq, c=c):
                    kb = (c * 4 + qb) % 2
                    for m in range(2):
                        o = m
                        r0 = SMALL[:, C_R0 + o:C_R0 + o + 1]
                        hq = 2 * c + m
                        P.op("dve", lambda e, m=m, r0=r0, hq=hq: e.tensor_tensor(
                            out=r0, in0=psO[:, jq, 256 * m + 64:256 * m + 65], in1=ESINK[:, hq:hq + 1], op=ALU.add),
                            (bO[jq][0], bSM), (bOW[o],))
                        P.op("dve", lambda e, r0=r0: e.reciprocal(out=r0, in_=r0), (bOW[o],), (bOW[o],))
                        P.op("dve", lambda e, m=m, r0=r0: e.tensor_scalar(
                            out=YB[kb][:, jq, 64 * m:64 * m + 64], in0=psO[:, jq, 256 * m:256 * m + 64], scalar1=r0,
                            scalar2=None, op0=ALU.mult), (bO[jq][0], bOW[o]), (bYB[kb],))
                    if jq == 3:
                        y_store(qb, c, 128, kb)

                attention_head(lambda m: QT[0], lambda m: KT[0], lambda m: 64 * m, 64,
                               lambda kt, kv=kv: VPs[:, kt, kv, :], 64, items_fn,
                               (lambda m, c=c: slopes[2 * c + m]), AUXQ[0], AUXK, 12, finalize,
                               nmaps=2, bq=bQT[0], bk=bKT[0], baux=bAUXQ[0])
        P.fence()
        phase_d(s, l, W["swa_w_out"])

    def layer_moba(s, l):
        w_in = W["moba_w_in"]
        slopes = alibi_slopes(16)
        scale = 128 ** -0.5
        phase_a(s, l)
        P.fence()
        P.op("dve", lambda e: e.memset(R2b[:, 0:16 * 16 * 129], 1.0), (), tuple(bVP))
        for j in range(8):
            def consume(i, pap, pbuf, j=j):
                copy_op(evac_engine(), VP[:, i, 2 * j:2 * j + 2, 0:128], pap.rearrange("p (h d) -> p h d", h=2),
                        (pbuf,), (bVP[i],))
            proj_tok(w_in, 4096 + 256 * j, 256, consume)
        proj_z(w_in, 6144)

        def items_fn(qb):
            its = []
            for kt in range(4 * qb + 4):
                if kt < 4 * qb:
                    its.append((0, kt, 0, [], 4))
                else:
                    its.append((0, kt, kt - 4 * qb, [CM], 4))
            return its
        items_fn.first_kt = lambda m, tq: 0

        for h in range(16):
            slot = st["ws"]
            st["ws"] ^= 1
            load_w(w_in, 128 * h, 128, 0, slot)
            load_w(w_in, 2048 + 128 * h, 128, 128, slot)
            proj_feat(slot, 0, 128, lambda tb: QT[0][:, 512 * tb:512 * tb + 512], bQT[0], scale=scale / slopes[h])
            proj_feat(slot, 128, 128, lambda tb: KT[0][:, 512 * tb:512 * tb + 512], bKT[0])
            P.op("dve", lambda e: e.reduce_sum(out=KMEAN[:], in_=KT[0][:, :].rearrange("p (n k) -> p n k", n=8),
                                               axis=AX.X), (bKT[0],), (bGate,))
            P.op("dve", lambda e: e.tensor_copy(out=KMEANB[:], in_=KMEAN[:]), (bGate,), (bGate,))
            for t in range(16):
                P.op("pe", lambda e, t=t: e.matmul(psG[0][:, 8 * t:8 * t + 8], lhsT=QT[0][:, 128 * t:128 * t + 128],
                                                   rhs=KMEANB[:], start=True, stop=True), (bQT[0], bGate), (bG[0],))
            P.op("dve", lambda e: e.tensor_tensor(out=GATE[:], in0=psG[0][:, 0:128].rearrange("p (t n) -> p t n", t=16),
                                                  in1=PASTM[:], op=ALU.add), (bG[0], bConst), (bGate,))
            for t in range(16):
                P.op("dve", lambda e, t=t: e.max(out=TOP8[:, t, :], in_=GATE[:, t, :]), (bGate,), (bMaskv,))
            P.op("dve", lambda e: e.tensor_tensor(out=GATE2[:], in0=GATE[:],
                                                  in1=TOP8[:, :, 2:3].to_broadcast([128, 16, 8]), op=ALU.is_ge),
                 (bGate, bMaskv), (bMaskv,))
            P.op("dve", lambda e: e.tensor_tensor(out=GATE2[:], in0=GATE2[:], in1=PAST01[:], op=ALU.mult),
                 (bMaskv, bConst), (bMaskv,))
            P.op("dve", lambda e: e.tensor_tensor(out=GATE2[:], in0=GATE2[:], in1=OWN01[:], op=ALU.add),
                 (bMaskv, bConst), (bMaskv,))
            P.op("dve", lambda e: e.tensor_scalar(out=MASKV[:], in0=GATE2[:], scalar1=BIG, scalar2=-BIG,
                                                  op0=ALU.mult, op1=ALU.add), (bMaskv,), (bMaskv,))
            for g in range(2):
                for j in range(8):
                    t = 8 * g + j
                    P.op("pe", lambda e, t=t, j=j: e.transpose(out=psT[0][0:8, j, :], in_=MASKV[:, t, :],
                                                               identity=IDENT[:]), (bMaskv, bConst), (bT[0],))
                copy_op(evac_engine(), AUXQ[0][0:8, 1024 * g:1024 * g + 1024].rearrange("p (j q) -> p j q", j=8),
                        psT[0][0:8, :, :], (bT[0],), (bAUXQ[0],))

            def finalize(qb, jq, h=h):
                o = jq % 2
                kb = (h * 4 + qb) % 2
                r0 = SMALL[:, C_R0 + o:C_R0 + o + 1]
                P.op("dve", lambda e: e.reciprocal(out=r0, in_=psO[:, jq, 128:129]), (bO[jq][0],), (bOW[o],))
                P.op("dve", lambda e: e.tensor_scalar(out=YB[kb][:, jq, :], in0=psO[:, jq, 0:128], scalar1=r0,
                                                      scalar2=None, op0=ALU.mult), (bO[jq][0], bOW[o]), (bYB[kb],))
                if jq == 3:
                    y_store(qb, h, 128, kb)

            attention_head(lambda m: QT[0], lambda m: KT[0], lambda m: 0, 128,
                           lambda kt, h=h: VP[:, kt, h, :], 128, items_fn, (lambda m, h=h: slopes[h]),
                           AUXQ[0], AUXK, 12, finalize, nmaps=1, bq=bQT[0], bk=bKT[0], baux=bAUXQ[0])
        P.fence()
        phase_d(s, l, W["moba_w_out"])

    def phase_d(s, l, w_out, s5=False):
        xsrc = x_in if l == LAYERS[0] else out
        if s5:
            Wd = R2b[:, 0:NC * 2048].rearrange("p (c t) -> p c t", c=NC)
            bWd = [Buf(f"wd{c}") for c in range(NC)]
            XTl = [STG[k][:, :, :].rearrange("p c n -> p (c n)") for k in range(2)]
            PGl = WS[0][:, :, :].rearrange("p c n -> p (c n)").bitcast(F32)
            TTl = WS[1][:, :, :].rearrange("p c n -> p (c n)").bitcast(F32)
            JUNKl = QT[0][:, :]
        else:
            Wd, bWd = R1, bR1
            XTl, PGl, TTl, JUNKl = XT, PG, TT, JUNK
        for nb in range(8):
            for g in range(2):
                load_w_to(w_out, 1024 * g, 256 * nb, 256,
                          lambda g=g, nb=nb: Wd[:, 8 * g:8 * g + 8, 256 * nb:256 * nb + 256],
                          tuple(bWd[8 * g:8 * g + 8]))
        if s5:
            P.fence()
            P.dma("sp", lambda e: e.dma_start(out=XTl[0], in_=post_g[l, :].partition_broadcast(128)), (), (bXT[0],))
            P.op("dve", lambda e: e.tensor_copy(out=PGl, in_=XTl[0]), (bXT[0],), (bPG,))
        else:
            P.dma("sp", lambda e: e.dma_start(out=PGl, in_=post_g[l, :].partition_broadcast(128)), (), (bPG,))
        def loads(i):
            k = i % 2
            P.dma("sp", lambda e, i=i, k=k: e.dma_start(out=XTl[k], in_=xsrc[s, 128 * i:128 * i + 128, :]),
                  (bX[s][i],), (bXT[k],))
            if not s5:
                P.dma("sp", lambda e, i=i, k=k: e.dma_start(out=Yt[k], in_=YS[128 * i:128 * i + 128, :]),
                      (bYd[i // 4],), (bYt[k],))
                P.dma("sp", lambda e, i=i, k=k: e.dma_start(out=SZt[k], in_=SZ[128 * i:128 * i + 128, :]),
                      (bSZd[i],), (bSZt[k],))

        loads(0)
        for i in range(NT):
            k = i % 2
            if i + 1 < NT:
                loads(i + 1)
            if not s5:
                P.op("dve", lambda e, k=k: e.tensor_tensor(out=YG, in0=Yt[k], in1=SZt[k], op=ALU.mult),
                     (bYt[k], bSZt[k]), (bYG,))
                for g in range(2):
                    for j in range(8):
                        c = 8 * g + j
                        P.op("pe", lambda e, c=c, j=j: e.transpose(out=psT[0][:, j, :], in_=YG[:, 128 * c:128 * c + 128],
                                                                   identity=IDENT[:]), (bYG, bConst), (bT[0],))
                    copy_op(evac_engine(), YGTv[:, 8 * g:8 * g + 8, :], psT[0][:, :, :], (bT[0],), (bYGT,))
            for nb in range(4):
                for c in range(NC):
                    if s5:
                        P.op("pe", lambda e, nb=nb, c=c, i=i: e.matmul(
                            psO[:, nb, :], lhsT=R1[:, c, 128 * i:128 * i + 128], rhs=Wd[:, c, 512 * nb:512 * nb + 512],
                            start=(c == 0), stop=(c == NC - 1)), (bR1[c], bWd[c]), (bOall,))
                    else:
                        P.op("pe", lambda e, nb=nb, c=c: e.matmul(psO[:, nb, :], lhsT=YGTv[:, c, :],
                                                                  rhs=Wd[:, c, 512 * nb:512 * nb + 512],
                                                                  start=(c == 0), stop=(c == NC - 1)),
                             (bYGT, bWd[c]), (bOall,))
            ss = SMALL[:, C_SS2 + 4 + k:C_SS2 + 5 + k]
            P.op("act", lambda e, ss=ss: e.activation(out=JUNKl.rearrange("p (a b) -> p a b", a=4), in_=psO[:, :, :],
                                                      func=AF.Square, accum_out=ss), (bOall,), (bJ, bSS[k]))
            P.op("dve", lambda e, ss=ss: e.tensor_scalar(out=ss, in0=ss, scalar1=1.0 / D, scalar2=EPS,
                                                         op0=ALU.mult, op1=ALU.add), (bSS[k],), (bSS[k],))
            P.op("act", lambda e, ss=ss: e.activation(out=ss, in_=ss, func=AF.Sqrt), (bSS[k],), (bSS[k],))
            P.op("dve", lambda e, ss=ss: e.reciprocal(out=ss, in_=ss), (bSS[k],), (bSS[k],))
            P.op("dve", lambda e, ss=ss: e.scalar_tensor_tensor(out=TTl.rearrange("p (a b) -> p a b", a=4),
                                                                in0=psO[:, :, :], scalar=ss,
                                                                in1=PGl.rearrange("p (a b) -> p a b", a=4),
                                                                op0=ALU.mult, op1=ALU.mult),
                 (bOall, bSS[k], bPG), (bTT,))
            P.op("pool", lambda e, k=k: e.tensor_tensor(out=XTl[k], in0=XTl[k], in1=TTl, op=ALU.add),
                 (bXT[k], bTT), (bXT[k],))
            P.dma("sp", lambda e, i=i, k=k: e.dma_start(out=out[s, 128 * i:128 * i + 128, :], in_=XTl[k]),
                  (bXT[k],), (bX[s][i],))
        P.fence()

    setup()
    for s in range(NSEQ):
        for l in LAYERS:
            if l == 0:
                layer_s5(s, l)
            elif l == 1:
                layer_diff(s, l)
            elif l == 2:
                layer_moba(s, l)
            elif l == 3:
                layer_swa(s, l)
            else:
                raise NotImplementedError(l)

    sem_keys = list(ENG) + [("d", d) for d in range(P.NDMA)]
    sems = {}
    for kx in sem_keys:
        nm = kx if isinstance(kx, str) else f"dma{kx[1]}"
        sems[kx] = es.enter_context(nc.semaphore("sem_" + nm))
    with nc.Block() as block:
        P.emit(nc, block, sems)
    es.close()
    return nc


def host_inputs(inp, lo, hi):
    f = np.ascontiguousarray
    m = {"x": f(inp["x"][lo:hi])}
    m["pre_gT"] = f(inp["pre_norm"].reshape(4, NC, 128).transpose(0, 2, 1))
    m["post_norm"] = f(inp["post_norm"])
    for k in ("s5_w_in", "s5_w_glu", "s5_w_out", "diff_w_in", "diff_w_out", "moba_w_in", "moba_w_out",
              "swa_w_in", "swa_w_out"):
        m[k] = f(inp[k][0])
    m["diff_l"] = f(np.stack([inp["diff_lq1"][0], inp["diff_lk1"][0], inp["diff_lq2"][0], inp["diff_lk2"][0]]))
    m["diff_subln"] = f(inp["diff_subln"])
    m["swa_sinks"] = f(inp["swa_sinks"])
    a_re, a_im, ldt = inp["s5_a_re"][0], inp["s5_a_im"][0], inp["s5_log_dt"][0]
    b_re, b_im, c_re, c_im = inp["s5_b_re"][0], inp["s5_b_im"][0], inp["s5_c_re"][0], inp["s5_c_im"][0]

    def xlay(a):
        return f(np.broadcast_to(a.reshape(16, 8, 1, 64), (16, 8, 16, 64)).transpose(1, 2, 0, 3).reshape(128, 16, 64))
    m["s5_are_X"] = xlay(a_re)
    m["s5_aim_X"] = xlay(a_im)
    m["s5_ldt_X"] = f(np.broadcast_to(ldt.reshape(16, 8, 1), (16, 8, 16)).transpose(1, 2, 0).reshape(128, 16))
    m["s5_bre_X"] = f(b_re.reshape(16, 8, 64, 16).transpose(1, 3, 0, 2).reshape(128, 16, 64))
    m["s5_bim_X"] = f(b_im.reshape(16, 8, 64, 16).transpose(1, 3, 0, 2).reshape(128, 16, 64))
    m["s5_are_Y"] = f(a_re.reshape(64, 2, 64).transpose(1, 2, 0).reshape(128, 64))
    m["s5_aim_Y"] = f(a_im.reshape(64, 2, 64).transpose(1, 2, 0).reshape(128, 64))
    m["s5_ldt_Y"] = f(np.broadcast_to(ldt.reshape(64, 2, 1), (64, 2, 64)).transpose(1, 2, 0).reshape(128, 64))
    m["s5_cre_Y"] = f(c_re.reshape(64, 2, 16, 64).transpose(1, 3, 0, 2).reshape(128, 64, 16))
    m["s5_cim_Y"] = f(c_im.reshape(64, 2, 16, 64).transpose(1, 3, 0, 2).reshape(128, 64, 16))
    m["s5_dT"] = f(inp["s5_d"][0].reshape(NC, 128).T)
    m["s5_b_gluT"] = f(inp["s5_b_glu"][0].reshape(NC, 128).T)
    return {k: np.asarray(v, dtype=np.float32) for k, v in m.items()}


_CACHE = {}


def kernel(**inputs):
    inputs = {k: np.asarray(v) for k, v in inputs.items()}
    n = 8
    per = inputs["x"].shape[0] // n
    key = ("full", per)
    if key not in _CACHE:
        _CACHE[key] = build_program(per, [0, 1, 2, 3])
    nc = _CACHE[key]
    in_maps = [host_inputs(inputs, c * per, (c + 1) * per) for c in range(n)]
    res = run_bass_kernel_spmd(nc, in_maps, core_ids=list(range(n)))
    return np.concatenate([np.asarray(r["out"], dtype=np.float32) for r in res.results], axis=0)
```
